# Optimizing a Trainium2 kernel written in Bass

```python
import jax
import jax.numpy as jnp
from jax import lax
import numpy as np

D_MODEL = 2048
BATCH = 8
SEQ = 2048
DEPTH = 1

GRID_W = 64
CTX_LEN = 256
MIX_GROUP = D_MODEL // 2
HEADS = 8
HEAD_DIM = MIX_GROUP // HEADS
N_PARTS = 8
P_QN, P_KN, P_VN, P_QH, P_FF, P_FB, P_IH, P_GH = range(N_PARTS)
WIN_R = 8
WIN_C = 16
Q_COLS = 16
K_COLS = 32
CHUNK = 64
ROPE_THETA = 10000.0
N_EXPERTS = 32
TOP_K = 4
MOE_FF = D_MODEL
SWIGLU_LIMIT = 7.0
SWIGLU_ALPHA = 1.702
MOE_BLOCK = 128
EPS = 1e-6

kernel_name = 'hybrid_natten_hgrn2_moe_block'


def rms_norm(x, gain):
    xf = x.astype(jnp.float32)
    y = xf * lax.rsqrt(jnp.mean(xf * xf, axis=-1, keepdims=True) + EPS)
    return (y * gain.astype(jnp.float32)).astype(x.dtype)


def modulate(h, shift, scale):
    return h * (1.0 + scale) + shift


def flip(a):
    return jnp.flip(a, axis=1)


def in_proj(h, w_in, parts):
    w = w_in.reshape(w_in.shape[0], N_PARTS, MIX_GROUP)[:, np.asarray(parts)]
    p = jnp.einsum('bld,dpe->pble', h, w)
    b, l = h.shape[0], h.shape[1]
    return {name: p[i].reshape(b, l, HEADS, HEAD_DIM) for i, name in enumerate(parts)}


def axial_rope_angles(l):
    t = jnp.arange(l)
    n_freq = HEAD_DIM // 4
    inv_freq = ROPE_THETA ** (-jnp.arange(n_freq, dtype=jnp.float32) / n_freq)
    ang_row = (t // GRID_W).astype(jnp.float32)[:, None] * inv_freq
    ang_col = (t % GRID_W).astype(jnp.float32)[:, None] * inv_freq
    return ang_row, ang_col


def rotate(x, ang):
    half = x.shape[-1] // 2
    cos = jnp.cos(ang)[:, None, :]
    sin = jnp.sin(ang)[:, None, :]
    x1, x2 = x[..., :half], x[..., half:]
    return jnp.concatenate([x1 * cos - x2 * sin, x2 * cos + x1 * sin], axis=-1)


def axial_rope(x, ang_row, ang_col):
    half = x.shape[-1] // 2
    return jnp.concatenate([rotate(x[..., :half], ang_row), rotate(x[..., half:], ang_col)], axis=-1)


def neighborhood_attention(q, k, v, k_ctx, v_ctx, rpb):
    b, l, h, dh = q.shape
    rows = l // GRID_W
    kr = min(WIN_R, rows)
    ncb = GRID_W // Q_COLS
    r = np.arange(rows)
    key_rows = np.clip(r - kr // 2, 0, rows - kr)[:, None] + np.arange(kr)
    kc0 = np.clip(np.arange(ncb) * Q_COLS - WIN_C // 2, 0, GRID_W - K_COLS)
    key_cols = kc0[:, None] + np.arange(K_COLS)
    nk = kr * K_COLS
    key_idx = (key_rows[:, None, :, None] * GRID_W + key_cols[None, :, None, :]).reshape(rows, ncb, nk)
    kg = k[:, key_idx]
    vg = v[:, key_idx]
    qb = q.reshape(b, rows, ncb, Q_COLS, h, dh)
    scale = dh ** -0.5
    s_win = jnp.einsum('brjqhd,brjkhd->bhrjqk', qb, kg).astype(jnp.float32) * scale
    q_cols = np.arange(GRID_W).reshape(ncb, Q_COLS)
    col_start = np.clip(q_cols - WIN_C // 2, 0, GRID_W - WIN_C)
    kcol = np.tile(key_cols, (1, kr))
    krow = np.repeat(key_rows, K_COLS, axis=1)
    in_win = (kcol[:, None, :] >= col_start[:, :, None]) & (kcol[:, None, :] < col_start[:, :, None] + WIN_C)
    d_row = krow - r[:, None] + WIN_R - 1
    d_col = np.clip(kcol[:, None, :] - q_cols[:, :, None] + WIN_C - 1, 0, 2 * WIN_C - 2)
    bias = rpb[:, d_row[:, None, None, :], d_col[None, :, :, :]].astype(jnp.float32)
    s_win = jnp.where(in_win[None, None, None], s_win + bias[None], -jnp.inf)
    s_ctx = jnp.einsum('brjqhd,bchd->bhrjqc', qb, k_ctx).astype(jnp.float32) * scale
    p = jax.nn.softmax(jnp.concatenate([s_win, s_ctx], axis=-1), axis=-1).astype(v.dtype)
    o = (jnp.einsum('bhrjqk,brjkhd->brjqhd', p[..., :nk], vg)
         + jnp.einsum('bhrjqc,bchd->brjqhd', p[..., nk:], v_ctx))
    return o.reshape(b, l, h, dh)


def context_attention(q, k, v):
    s = jnp.einsum('bqhd,bkhd->bhqk', q, k).astype(jnp.float32) * (q.shape[-1] ** -0.5)
    p = jax.nn.softmax(s, axis=-1).astype(v.dtype)
    return jnp.einsum('bhqk,bkhd->bqhd', p, v)


def forget_gate(f_raw, lb):
    f_raw = f_raw.astype(jnp.float32)
    log_f = jnp.log(lb + (1.0 - lb) * jax.nn.sigmoid(f_raw))
    key = (1.0 - lb) * jax.nn.sigmoid(-f_raw)
    return log_f, key


def gla_chunk_scan(q, k, v, g, s0):
    b, l, h, dk = q.shape
    dv = v.shape[-1]
    n = l // CHUNK
    mask = jnp.tril(jnp.ones((CHUNK, CHUNK), dtype=bool))[None, :, :, None, None]

    def to_chunks(a):
        return jnp.moveaxis(a.reshape(b, n, CHUNK, h, a.shape[-1]), 1, 0)

    def step(s, inp):
        qc, kc, vc, gc = inp
        cum = jnp.cumsum(gc, axis=1)
        cum_last = cum[:, -1]
        o_inter = jnp.einsum('bthk,bhkv->bthv', qc * jnp.exp(cum), s)
        decay = jnp.exp(jnp.where(mask, cum[:, :, None] - cum[:, None, :], -jnp.inf))
        att = jnp.sum(qc[:, :, None] * kc[:, None] * decay, axis=-1)
        o_intra = jnp.einsum('btsh,bshv->bthv', att, vc)
        k_dec = kc * jnp.exp(cum_last[:, None] - cum)
        s_new = jnp.exp(cum_last)[..., None] * s + jnp.einsum('bshk,bshv->bhkv', k_dec, vc)
        return s_new, o_inter + o_intra

    s_final, o = lax.scan(step, s0, (to_chunks(q), to_chunks(k), to_chunks(v), to_chunks(g)))
    return jnp.moveaxis(o, 0, 1).reshape(b, l, h, dv), s_final


def gla_final_state(k, v, g):
    cum = jnp.cumsum(g, axis=1)
    return jnp.einsum('blhk,blhv->bhkv', k * jnp.exp(cum[:, -1:] - cum), v)


def hgrn2_output(o, gate, gain, dtype):
    return (rms_norm(o, gain) * jax.nn.silu(gate.astype(jnp.float32))).astype(dtype)


def hybrid_mixer(hx, hc, w_in, lb, hg_gain, rpb, w_out, ang_row, ang_col, with_ctx_out):
    b, l, _ = hx.shape
    px = in_proj(hx, w_in, tuple(range(N_PARTS)))
    ctx_parts = tuple(range(N_PARTS)) if with_ctx_out else (P_KN, P_VN, P_FF, P_FB, P_IH)
    pc = in_proj(hc, w_in, ctx_parts)
    o_na = neighborhood_attention(px[P_QN], px[P_KN], px[P_VN], pc[P_KN], pc[P_VN], rpb)
    lb_f, lb_b = lb.reshape(2, HEADS, HEAD_DIM)
    gf_c, kf_c = forget_gate(pc[P_FF], lb_f)
    gb_c, kb_c = forget_gate(pc[P_FB], lb_b)
    v_c = pc[P_IH].astype(jnp.float32)
    if with_ctx_out:
        q_c = pc[P_QH].astype(jnp.float32)
        zero = jnp.zeros((b, HEADS, HEAD_DIM, HEAD_DIM), jnp.float32)
        oc_f, s_f = gla_chunk_scan(q_c, kf_c, v_c, gf_c, zero)
        oc_b, s_b = gla_chunk_scan(flip(q_c), flip(kb_c), flip(v_c), flip(gb_c), zero)
        oc_hg = oc_f + flip(oc_b)
    else:
        s_f = gla_final_state(kf_c, v_c, gf_c)
        s_b = gla_final_state(flip(kb_c), flip(v_c), flip(gb_c))
    gf, kf = forget_gate(px[P_FF], lb_f)
    gb, kb = forget_gate(px[P_FB], lb_b)
    q_x = axial_rope(px[P_QH].astype(jnp.float32), ang_row, ang_col)
    kf = axial_rope(kf, ang_row, ang_col)
    kb = axial_rope(kb, ang_row, ang_col)
    v_x = px[P_IH].astype(jnp.float32)
    o_f, _ = gla_chunk_scan(q_x, kf, v_x, gf, s_f)
    o_b, _ = gla_chunk_scan(flip(q_x), flip(kb), flip(v_x), flip(gb), s_b)
    o_hg = hgrn2_output(o_f + flip(o_b), px[P_GH], hg_gain, hx.dtype)
    y = jnp.concatenate([o_na.reshape(b, l, MIX_GROUP), o_hg.reshape(b, l, MIX_GROUP)], axis=-1) @ w_out
    if not with_ctx_out:
        return y, None
    lc = hc.shape[1]
    oc_na = context_attention(pc[P_QN], pc[P_KN], pc[P_VN])
    oc_hg = hgrn2_output(oc_hg, pc[P_GH], hg_gain, hc.dtype)
    yc = jnp.concatenate([oc_na.reshape(b, lc, MIX_GROUP), oc_hg.reshape(b, lc, MIX_GROUP)], axis=-1) @ w_out
    return y, yc


def moe_ffn(h, w_router, b_router, w_gu, b_gu, w_down, b_down):
    t, d = h.shape
    logits = (h @ w_router + b_router).astype(jnp.float32)
    top_val, top_idx = lax.top_k(logits, TOP_K)
    gate = jax.nn.softmax(top_val, axis=-1)
    a = t * TOP_K
    exp_flat = top_idx.reshape(a)
    order = jnp.argsort(exp_flat)
    e_sorted = exp_flat[order]
    tok_sorted = (order // TOP_K).astype(jnp.int32)
    w_sorted = gate.reshape(a)[order].astype(h.dtype)
    counts = jnp.bincount(exp_flat, length=N_EXPERTS)
    padded = (counts + MOE_BLOCK - 1) // MOE_BLOCK * MOE_BLOCK
    start = jnp.cumsum(counts) - counts
    pad_end = jnp.cumsum(padded)
    pad_start = pad_end - padded
    dest = pad_start[e_sorted] + (jnp.arange(a) - start[e_sorted])
    n_blocks = -(-(a + N_EXPERTS * (MOE_BLOCK - 1)) // MOE_BLOCK)
    cap = n_blocks * MOE_BLOCK
    slot_tok = jnp.full((cap,), t, jnp.int32).at[dest].set(tok_sorted)
    slot_w = jnp.zeros((cap,), h.dtype).at[dest].set(w_sorted)
    block_exp = jnp.minimum(jnp.searchsorted(pad_end, jnp.arange(n_blocks) * MOE_BLOCK, side='right'), N_EXPERTS - 1)
    h_pad = jnp.concatenate([h, jnp.zeros((1, d), h.dtype)], axis=0)
    xb = h_pad[slot_tok].reshape(n_blocks, MOE_BLOCK, d)

    def expert_block(args):
        xe, e = args
        gu = xe @ w_gu[e] + b_gu[e]
        x_glu = jnp.minimum(gu[:, :MOE_FF], SWIGLU_LIMIT)
        x_lin = jnp.clip(gu[:, MOE_FF:], -SWIGLU_LIMIT, SWIGLU_LIMIT)
        y = x_glu * jax.nn.sigmoid(SWIGLU_ALPHA * x_glu) * (x_lin + 1.0)
        return y @ w_down[e] + b_down[e]

    yb = lax.map(expert_block, (xb, block_exp)).reshape(cap, d)
    out = jnp.zeros((t + 1, d), h.dtype).at[slot_tok].add(yb * slot_w[:, None])
    return out[:t]


def setup_inputs(seed: int = 0) -> dict:
    key = jax.random.key(seed)
    ks = jax.random.split(key, 20)
    d = D_MODEL

    def nrm(k, shape, s):
        return jax.random.normal(k, shape, jnp.float32) * s

    return {
        'x': nrm(ks[0], (BATCH, SEQ, d), 1.0),
        'c': nrm(ks[1], (BATCH, d), 1.0),
        'ctx': nrm(ks[2], (BATCH, CTX_LEN, d), 1.0),
        'c_ctx': nrm(ks[3], (d,), 1.0),
        'w_ada': nrm(ks[4], (DEPTH, d, 6 * d), 0.5 * d ** -0.5),
        'b_ada': nrm(ks[5], (DEPTH, 6 * d), 0.02),
        'norm_mix': 1.0 + nrm(ks[6], (DEPTH, d), 0.02),
        'norm_ffn': 1.0 + nrm(ks[7], (DEPTH, d), 0.02),
        'w_in': nrm(ks[8], (DEPTH, d, N_PARTS * MIX_GROUP), d ** -0.5),
        'lb_table': nrm(ks[9], (DEPTH + 1, 2 * MIX_GROUP), 0.5),
        'hg_norm': 1.0 + nrm(ks[10], (DEPTH, HEAD_DIM), 0.02),
        'rpb': nrm(ks[11], (DEPTH, HEADS, 2 * WIN_R - 1, 2 * WIN_C - 1), 0.02),
        'w_out': nrm(ks[12], (DEPTH, 2 * MIX_GROUP, d), (2 * MIX_GROUP) ** -0.5),
        'w_router': nrm(ks[13], (DEPTH, d, N_EXPERTS), d ** -0.5),
        'b_router': nrm(ks[14], (DEPTH, N_EXPERTS), 0.01),
        'w_gu': nrm(ks[15], (DEPTH, N_EXPERTS, d, 2 * MOE_FF), d ** -0.5),
        'b_gu': nrm(ks[16], (DEPTH, N_EXPERTS, 2 * MOE_FF), 0.01),
        'w_down': nrm(ks[17], (DEPTH, N_EXPERTS, MOE_FF, d), MOE_FF ** -0.5),
        'b_down': nrm(ks[18], (DEPTH, N_EXPERTS, d), 0.01),
        'norm_final': 1.0 + nrm(ks[19], (d,), 0.02),
    }


def reference(x, c, ctx, c_ctx, w_ada, b_ada, norm_mix, norm_ffn, w_in, lb_table, hg_norm, rpb,
              w_out, w_router, b_router, w_gu, b_gu, w_down, b_down, norm_final):
    b, l, d = x.shape
    lc = ctx.shape[1]
    ang_row, ang_col = axial_rope_angles(l)
    lower_bounds = jnp.cumsum(jax.nn.softmax(lb_table.astype(jnp.float32), axis=0), axis=0)
    cond = jax.nn.silu(c)
    cond_ctx = jax.nn.silu(c_ctx)
    h_ctx = ctx
    for layer in range(DEPTH):
        last = layer == DEPTH - 1
        mod = cond @ w_ada[layer] + b_ada[layer]
        sh1, sc1, g1, sh2, sc2, g2 = jnp.split(mod[:, None, :], 6, axis=-1)
        n_ctx_mod = 2 if last else 6
        mod_ctx = jnp.split(cond_ctx @ w_ada[layer][:, :n_ctx_mod * d] + b_ada[layer][:n_ctx_mod * d], n_ctx_mod)
        hx = modulate(rms_norm(x, norm_mix[layer]), sh1, sc1)
        hc = modulate(rms_norm(h_ctx, norm_mix[layer]), mod_ctx[0], mod_ctx[1])
        y, yc = hybrid_mixer(hx, hc, w_in[layer], lower_bounds[layer], hg_norm[layer], rpb[layer],
                             w_out[layer], ang_row, ang_col, not last)
        x = x + g1 * y
        hx = modulate(rms_norm(x, norm_ffn[layer]), sh2, sc2)
        moe_params = (w_router[layer], b_router[layer], w_gu[layer], b_gu[layer], w_down[layer], b_down[layer])
        if last:
            x = x + g2 * moe_ffn(hx.reshape(b * l, d), *moe_params).reshape(b, l, d)
        else:
            h_ctx = h_ctx + mod_ctx[2] * yc
            hc = modulate(rms_norm(h_ctx, norm_ffn[layer]), mod_ctx[3], mod_ctx[4])
            tokens = jnp.concatenate([hc.reshape(b * lc, d), hx.reshape(b * l, d)], axis=0)
            out = moe_ffn(tokens, *moe_params)
            h_ctx = h_ctx + mod_ctx[5] * out[:b * lc].reshape(b, lc, d)
            x = x + g2 * out[b * lc:].reshape(b, l, d)
    return rms_norm(x, norm_final)
```

```python
import numpy as np
from contextlib import ExitStack
import concourse.bass as bass
import concourse.mybir as mybir
from concourse.bass_utils import run_bass_kernel_spmd

F32 = mybir.dt.float32
BF16 = mybir.dt.bfloat16
I32 = mybir.dt.int32
U32 = mybir.dt.uint32
AF = mybir.ActivationFunctionType
ALU = mybir.AluOpType
AX = mybir.AxisListType

ENG_NAMES = ("pe", "act", "dve", "pool", "sp")
N_DMA_SEMS = 6

D = 2048
T = 2048
LC = 256
NH = 8
DH = 128
NE = 32
CAP = 768
CH = 32
EPS = 1e-6
NEG = -30000.0
import os
CUT = int(os.environ.get('KCUT', '99'))


class _Cut(Exception):
    pass


_PROG = [None]


def cut(n):
    if CUT == n:
        _PROG[0].disabled = True


class Prog:
    def __init__(self, nc, st):
        self.nc = nc
        self.thunks = {e: [] for e in ENG_NAMES}
        self.count = {e: 0 for e in ENG_NAMES}
        self.waited = {e: {} for e in ENG_NAMES}
        self.bufs = {}
        self.dma_rr = {e: 0 for e in ENG_NAMES}
        self.dma_val = {}
        self.semkeys = list(ENG_NAMES)
        for q in ("sp", "act", "pool"):
            for j in range(N_DMA_SEMS):
                k = "d_%s_%d" % (q, j)
                self.semkeys.append(k)
                self.dma_val[k] = 0
        self.sems = {}
        for k in self.semkeys:
            self.sems[k] = st.enter_context(nc.semaphore("s_" + k))
        self.ninstr = 0
        self.disabled = False
        _PROG[0] = self

    def _deps(self, reads, writes):
        deps = {}

        def add(pid):
            if pid is None:
                return
            k, v = pid
            if deps.get(k, 0) < v:
                deps[k] = v
        for key in reads:
            b = self.bufs.get(key)
            if b is not None:
                add(b[0])
        for key in writes:
            b = self.bufs.get(key)
            if b is not None:
                add(b[0])
                for r in b[1]:
                    add(r)
        return deps

    def _update(self, pid, reads, writes):
        for key in reads:
            b = self.bufs.setdefault(key, [None, []])
            b[1].append(pid)
            if len(b[1]) > 48:
                m = {}
                for (k, v) in b[1]:
                    if m.get(k, 0) < v:
                        m[k] = v
                b[1] = list(m.items())
        for key in writes:
            self.bufs[key] = [pid, []]

    def _waits_for(self, eng, deps):
        out = []
        w = self.waited[eng]
        for k, v in deps.items():
            if w.get(k, 0) < v:
                w[k] = v
                out.append((k, v))
        return out

    def op(self, eng, fn, reads=(), writes=()):
        if self.disabled:
            return None
        deps = self._deps(reads, writes)
        waits = self._waits_for(eng, deps)
        self.count[eng] += 1
        pid = (eng, self.count[eng])
        self._update(pid, reads, writes)
        sems = self.sems

        def thunk(e):
            for k, v in waits:
                e.wait_ge(sems[k], v)
            ins = fn(e)
            ins.then_inc(sems[eng], 1)
        self.thunks[eng].append(thunk)
        self.ninstr += 1
        return pid

    def dma(self, q, fn, reads=(), writes=()):
        if self.disabled:
            return None
        j = self.dma_rr[q]
        self.dma_rr[q] = (j + 1) % N_DMA_SEMS
        k = "d_%s_%d" % (q, j)
        deps = self._deps(reads, writes)
        prev = self.dma_val[k]
        if prev > 0 and deps.get(k, 0) < prev:
            deps[k] = prev
        waits = self._waits_for(q, deps)
        self.dma_val[k] = prev + 16
        pid = (k, prev + 16)
        self._update(pid, reads, writes)
        sems = self.sems

        def thunk(e):
            for kk, v in waits:
                e.wait_ge(sems[kk], v)
            ins = fn(e)
            ins.then_inc(sems[k], 16)
        self.thunks[q].append(thunk)
        self.ninstr += 1
        return pid

    def barrier(self):
        deps = {}
        for e in ENG_NAMES:
            if self.count[e] > 0:
                deps[e] = self.count[e]
        for k, v in self.dma_val.items():
            if v > 0:
                deps[k] = v
        sems = self.sems
        for eng in ENG_NAMES:
            waits = self._waits_for(eng, dict(deps))

            def thunk(e, waits=waits):
                for k, v in waits:
                    e.wait_ge(sems[k], v)
            self.thunks[eng].append(thunk)
        self.bufs = {}

    def emit_block(self):
        nc = self.nc
        th = self.thunks
        with nc.Block() as block:
            @block.tensor
            def _(e):
                for t in th["pe"]:
                    t(e)

            @block.scalar
            def _(e):
                for t in th["act"]:
                    t(e)

            @block.vector
            def _(e):
                for t in th["dve"]:
                    t(e)

            @block.gpsimd
            def _(e):
                for t in th["pool"]:
                    t(e)

            @block.sync
            def _(e):
                for t in th["sp"]:
                    t(e)
        self.thunks = {e: [] for e in ENG_NAMES}


def _host_consts():
    c = {}
    c["ident"] = np.eye(128, dtype=np.float32)
    perm = np.zeros((128, 128), np.float32)
    for m in range(128):
        perm[m ^ 32, m] = 1.0
    c["perm"] = perm
    t = np.arange(T)
    nf = DH // 4
    inv = (10000.0 ** (-np.arange(nf, dtype=np.float32) / nf)).astype(np.float32)
    ang_row = (t // 64).astype(np.float32)[:, None] * inv[None, :]
    ang_col = (t % 64).astype(np.float32)[:, None] * inv[None, :]
    C = np.zeros((128, T), np.float32)
    S = np.zeros((128, T), np.float32)
    for blk, ang in ((0, ang_row), (64, ang_col)):
        cs = np.cos(ang).astype(np.float32).T
        sn = np.sin(ang).astype(np.float32).T
        C[blk:blk + 32] = cs
        C[blk + 32:blk + 64] = cs
        S[blk:blk + 32] = -sn
        S[blk + 32:blk + 64] = sn
    c["ropeC"] = C
    c["ropeS"] = S
    qc = np.arange(64)
    col_start = np.clip(qc - 8, 0, 48)
    kc = np.arange(64)
    inw = (kc[:, None] >= col_start[None, :]) & (kc[:, None] < col_start[None, :] + 16)
    m = np.where(inw, 0.0, NEG).astype(np.float32)
    c["namask"] = np.concatenate([m, m], axis=0)
    s_ = np.arange(CH)
    mf = (s_[:, None] <= s_[None, :]).astype(np.float32)
    mb = (s_[:, None] >= s_[None, :]).astype(np.float32)
    a_ = np.arange(128)
    same = (a_[:, None] // CH) == (a_[None, :] // CH)
    bf_ = (same & (a_[:, None] <= a_[None, :])).astype(np.float32)
    bb_ = (same & (a_[:, None] >= a_[None, :])).astype(np.float32)
    c["trif"] = np.ascontiguousarray(np.broadcast_to(bf_[:, None, :], (128, 4, 128)))
    c["trib"] = np.ascontiguousarray(np.broadcast_to(bb_[:, None, :], (128, 4, 128)))
    c["iotae"] = np.ascontiguousarray(np.broadcast_to(np.arange(NE, dtype=np.float32)[None, :], (128, NE)))
    k_ = np.arange(128)
    c["lstrict"] = (k_[:, None] < k_[None, :]).astype(np.float32)
    return c


def _na_gather_idx():
    j = np.arange(2)[:, None, None, None]
    kc = np.arange(64)[None, :, None, None]
    d = np.arange(14)[None, None, :, None]
    qc = np.arange(64)[None, None, None, :]
    drow = np.broadcast_to(d + j, (2, 64, 14, 64)).reshape(128, 14, 64)
    dcol = np.broadcast_to(np.clip(kc - qc + 15, 0, 30), (2, 64, 14, 64)).reshape(128, 14, 64)
    return drow, dcol


def build(dbg=None, stop_after=None):
    dbg = dbg or {}
    nc = bass.Bass("TRN2", target_bir_lowering=False)

    def din(name, shape, dt=F32):
        return nc.dram_tensor(name, list(shape), dt, kind="ExternalInput").ap()

    x_d = din("x", [T, D])
    ctx_d = din("ctx", [LC, D])
    rows_d = din("rows8", [8, D])
    w_ada_d = din("w_ada", [D, 6 * D])
    b_ada_d = din("b_ada", [1, 6 * D])
    nffn_d = din("norm_ffn", [1, D])
    nfin_d = din("norm_final", [1, D])
    w_in_d = din("w_in", [D, 8 * 1024])
    hgn_d = din("hg_norm", [128, 1])
    nag_d = din("nag", [NH, 128, 2, 7, 64])
    w_out_d = din("w_out", [D, D])
    w_r_d = din("w_router", [D, NE])
    b_r_d = din("b_router", [1, NE])
    need_moe = stop_after in (None, 'F', 'F0', 'G')
    if need_moe:
        w_gu_d = din("w_gu", [NE, D, 2 * D])
        b_gu_d = din("b_gu", [NE, 2 * D])
        w_dn_d = din("w_down", [NE, D, D])
        b_dn_d = din("b_down", [NE, D])
    ident_d = din("ident", [128, 128])
    perm_d = din("perm", [128, 128])
    ropeC_d = din("ropeC", [128, T])
    ropeS_d = din("ropeS", [128, T])
    namask_d = din("namask", [128, 64])
    trif_d = din("trif", [128, 4, 128])
    trib_d = din("trib", [128, 4, 128])
    iotae_d = din("iotae", [128, NE])
    lstrict_d = din("lstrict", [128, 128])
    out_d = nc.dram_tensor("out", [T, D], F32, kind="ExternalOutput").ap()
    dbg_d = {k: nc.dram_tensor("dbg_" + k, list(shp[0]), shp[1], kind="ExternalOutput").ap() for k, shp in dbg.items()}

    bc_d = nc.dram_tensor("bc_scr", [5, D], F32, kind="Internal").ap()
    cat_d = nc.dram_tensor("cat_scr", [16, 128, T], BF16, kind="Internal").ap()
    x1_d = nc.dram_tensor("x1_scr", [T, D], F32, kind="Internal").ap()
    xs_d = nc.dram_tensor("xs_scr", [NE * CAP + 128, D], BF16, kind="Internal").ap()
    ys_d = nc.dram_tensor("ys_scr", [NE * CAP + 128, D], F32, kind="Internal").ap()

    with ExitStack() as top:
        P = Prog(nc, top)

        def sb(st, name, shape, dt=F32):
            return st.enter_context(nc.sbuf_tensor("sb_" + name, list(shape), dt))

        def ps(st, name, shape, dt=F32):
            return st.enter_context(nc.psum_tensor("ps_" + name, list(shape), dt))

        identf = sb(top, "identf", [128, 128])
        identb = sb(top, "identb", [128, 128], BF16)
        onesb = sb(top, "onesb", [128, 128], BF16)
        onesf = sb(top, "onesf", [128, 128])
        vecF = sb(top, "vecF", [128, 16, 8])
        modF = sb(top, "modF", [128, 96, 2])
        A1 = sb(top, "A1", [128, 16])
        Ac = sb(top, "Ac", [128, 16])
        lbF = sb(top, "lbF", [128, 16])
        omlF = sb(top, "omlF", [128, 16])
        nomlF = sb(top, "nomlF", [128, 16])
        hgF = sb(top, "hgF", [128, 1])

        P.dma("sp", lambda e: e.dma_start(out=identf[:], in_=ident_d), writes=["identf"])
        P.dma("pool", lambda e: e.dma_start(out=identb[:], in_=ident_d), writes=["identb"])
        P.op("dve", lambda e: e.memset(onesb[:], 1.0), writes=["onesb"])
        P.op("dve", lambda e: e.memset(onesf[:], 1.0), writes=["onesf"])
        P.dma("sp", lambda e: e.dma_start(out=hgF[:], in_=hgn_d), writes=["hgF"])

        def debug_out(name, ap, key):
            if name in dbg_d:
                P.dma("sp", lambda e: e.dma_start(out=dbg_d[name], in_=ap), reads=[key], writes=["dbg_" + name])

        with ExitStack() as st:
            rows8 = sb(st, "rows8", [8, D])
            sT = sb(st, "sT", [128, 16, 2])
            tmpA = sb(st, "tmpA", [128, 16, 2])
            mod_row = sb(st, "mod_row", [2, 6 * D])
            wa = [sb(st, "wa%d" % i, [128, 16, 512]) for i in range(2)]
            nrow = sb(st, "nrow", [1, D])
            a2row = sb(st, "a2row", [1, D])
            ps_v = ps(st, "ps_v", [128, 16, 8])
            ps_m = [ps(st, "ps_m%d" % i, [2, 512]) for i in range(2)]
            ps_f = ps(st, "ps_f", [128, 96, 2])

            P.dma("sp", lambda e: e.dma_start(out=rows8[:], in_=rows_d), writes=["rows8"])
            cut(0)
            P.dma("sp", lambda e: e.dma_start(out=mod_row[0:1, :], in_=b_ada_d), writes=["mod_row"])
            P.dma("sp", lambda e: e.dma_start(out=mod_row[1:2, :], in_=b_ada_d), writes=["mod_row"])
            P.dma("sp", lambda e: e.dma_start(out=nrow[:], in_=nffn_d), writes=["nrow"])

            def tr_rows(e):
                ins = None
                for k in range(16):
                    ins = e.transpose(ps_v[:, k, :], rows8[0:8, k * 128:(k + 1) * 128], identf[0:8, 0:8])
                return ins
            P.op("pe", tr_rows, reads=["rows8", "identf"], writes=["ps_v"])
            P.op("dve", lambda e: e.tensor_copy(vecF[:], ps_v[:]), reads=["ps_v"], writes=["vecF"])
            cut(1)
            P.op("act", lambda e: e.activation(tmpA[:], vecF[:, :, 3:5], AF.Exp, scale=-1.0), reads=["vecF"], writes=["tmpA"])
            P.op("dve", lambda e: e.tensor_scalar(tmpA[:], tmpA[:], 1.0, None, ALU.add), reads=["tmpA"], writes=["tmpA"])
            P.op("dve", lambda e: e.reciprocal(tmpA[:], tmpA[:]), reads=["tmpA"], writes=["tmpA"])
            P.op("dve", lambda e: e.tensor_tensor(sT[:], tmpA[:], vecF[:, :, 3:5], ALU.mult), reads=["tmpA", "vecF"], writes=["sT"])
            P.op("dve", lambda e: e.tensor_tensor(lbF[:], vecF[:, :, 2], vecF[:, :, 1], ALU.subtract), reads=["vecF"], writes=["lbF"])
            P.op("act", lambda e: e.activation(lbF[:], lbF[:], AF.Exp), reads=["lbF"], writes=["lbF"])
            P.op("dve", lambda e: e.tensor_scalar(lbF[:], lbF[:], 1.0, None, ALU.add), reads=["lbF"], writes=["lbF"])
            P.op("dve", lambda e: e.reciprocal(lbF[:], lbF[:]), reads=["lbF"], writes=["lbF"])
            P.op("dve", lambda e: e.tensor_scalar(omlF[:], lbF[:], -1.0, 1.0, ALU.mult, ALU.add), reads=["lbF"], writes=["omlF"])
            P.op("dve", lambda e: e.tensor_scalar(nomlF[:], lbF[:], -1.0, None, ALU.add), reads=["lbF"], writes=["nomlF"])

            cut(2)
            wav = w_ada_d.rearrange("(k p) n -> p k n", p=128)
            for n in range(24):
                bi = n % 2
                P.dma("sp", lambda e, n=n, bi=bi: e.dma_start(out=wa[bi][:], in_=wav[:, :, n * 512:(n + 1) * 512]), writes=["wa%d" % bi])

                def mmA(e, bi=bi):
                    ins = None
                    for k in range(16):
                        ins = e.matmul(ps_m[bi][:], sT[:, k, :], wa[bi][:, k, :], start=(k == 0), stop=(k == 15))
                    return ins
                P.op("pe", mmA, reads=["sT", "wa%d" % bi], writes=["ps_m%d" % bi])
                P.op("dve", lambda e, n=n, bi=bi: e.tensor_tensor(mod_row[:, n * 512:(n + 1) * 512], ps_m[bi][:], mod_row[:, n * 512:(n + 1) * 512], ALU.add),
                     reads=["ps_m%d" % bi, "mod_row"], writes=["mod_row"])

            cut(3)

            def tr_mod(e):
                ins = None
                for c in range(96):
                    ins = e.transpose(ps_f[:, c, :], mod_row[0:2, c * 128:(c + 1) * 128], identf[0:2, 0:2])
                return ins
            P.op("pe", tr_mod, reads=["mod_row", "identf"], writes=["ps_f"])
            P.op("dve", lambda e: e.tensor_copy(modF[:], ps_f[:]), reads=["ps_f"], writes=["modF"])
            P.op("dve", lambda e: e.scalar_tensor_tensor(A1[:], modF[:, 16:32, 0], 1.0, vecF[:, :, 0], ALU.add, ALU.mult), reads=["modF", "vecF"], writes=["A1"])
            P.op("dve", lambda e: e.scalar_tensor_tensor(Ac[:], modF[:, 16:32, 1], 1.0, vecF[:, :, 0], ALU.add, ALU.mult), reads=["modF", "vecF"], writes=["Ac"])
            cut(4)
            P.op("dve", lambda e: e.scalar_tensor_tensor(a2row[:], mod_row[0:1, 4 * D:5 * D], 1.0, nrow[:], ALU.add, ALU.mult), reads=["mod_row", "nrow"], writes=["a2row"])
            P.dma("sp", lambda e: e.dma_start(out=bc_d[0:1, :], in_=mod_row[0:1, 2 * D:3 * D]), reads=["mod_row"], writes=["bc_d"])
            P.dma("sp", lambda e: e.dma_start(out=bc_d[1:2, :], in_=a2row[:]), reads=["a2row"], writes=["bc_d"])
            P.dma("sp", lambda e: e.dma_start(out=bc_d[2:3, :], in_=mod_row[0:1, 3 * D:4 * D]), reads=["mod_row"], writes=["bc_d"])
            P.dma("sp", lambda e: e.dma_start(out=bc_d[3:4, :], in_=mod_row[0:1, 5 * D:6 * D]), reads=["mod_row"], writes=["bc_d"])
            debug_out("mod", mod_row[:], "mod_row")
            P.disabled = False
            P.barrier()
            P.emit_block()
        if stop_after == "A":
            return nc

        with ExitStack() as stBC:
            hxT = sb(stBC, "hxT", [128, 16, T], BF16)
            hcT = sb(stBC, "hcT", [128, 16, LC], BF16)
            with ExitStack() as st:
                xt = [sb(st, "xt%d" % i, [128, D]) for i in range(2)]
                xn = [sb(st, "xn%d" % i, [128, D]) for i in range(2)]
                junk = sb(st, "junk", [128, D], BF16)
                ss = [sb(st, "ss%d" % i, [128, 1]) for i in range(2)]
                tp = [ps(st, "tp%d" % i, [128, 4, 128]) for i in range(4)]
                tpi = 0
                for i in range(18):
                    bi = i % 2
                    isx = i < 16
                    src = x_d[i * 128:(i + 1) * 128, :] if isx else ctx_d[(i - 16) * 128:(i - 15) * 128, :]
                    dstT = hxT if isx else hcT
                    c0 = (i if isx else i - 16) * 128
                    Asc, bcol = (A1, 0) if isx else (Ac, 1)
                    P.dma("sp", lambda e, bi=bi, src=src: e.dma_start(out=xt[bi][:], in_=src), writes=["xt%d" % bi])
                    P.op("act", lambda e, bi=bi: e.activation(junk[:], xt[bi][:], AF.Square, accum_out=ss[bi][:]), reads=["xt%d" % bi], writes=["junk", "ss%d" % bi])
                    cut(10)
                    P.op("act", lambda e, bi=bi: e.activation(ss[bi][:], ss[bi][:], AF.Ln, scale=1.0 / D, bias=EPS), reads=["ss%d" % bi], writes=["ss%d" % bi])
                    P.op("act", lambda e, bi=bi: e.activation(ss[bi][:], ss[bi][:], AF.Exp, scale=-0.5), reads=["ss%d" % bi], writes=["ss%d" % bi])
                    cut(11)
                    P.op("dve", lambda e, bi=bi: e.tensor_scalar(xn[bi][:], xt[bi][:], ss[bi][:, 0:1], None, ALU.mult), reads=["xt%d" % bi, "ss%d" % bi], writes=["xn%d" % bi])
                    cut(12)
                    for kg in range(4):
                        tb = tpi % 4
                        tpi += 1

                        def trB(e, bi=bi, kg=kg, tb=tb):
                            ins = None
                            for kk in range(4):
                                k = kg * 4 + kk
                                ins = e.transpose(tp[tb][:, kk, :], xn[bi][:, k * 128:(k + 1) * 128], identf[:])
                            return ins
                        P.op("pe", trB, reads=["xn%d" % bi, "identf"], writes=["tp%d" % tb])
                        cut(13)
                        for kk in range(4):
                            k = kg * 4 + kk
                            if True:
                                P.op("act", lambda e, tb=tb, kk=kk, k=k, dstT=dstT, c0=c0, Asc=Asc, bcol=bcol: e.activation(
                                    dstT[:, k, c0:c0 + 128], tp[tb][:, kk, :], AF.Identity, scale=Asc[:, k:k + 1], bias=modF[:, k, bcol:bcol + 1]),
                                    reads=["tp%d" % tb, "A1", "Ac", "modF"], writes=["hT%d_%d" % (i, k)])
                                cut(14)
                            else:
                                P.op("dve", lambda e, tb=tb, kk=kk, k=k, dstT=dstT, c0=c0, Asc=Asc, bcol=bcol: e.tensor_scalar(
                                    dstT[:, k, c0:c0 + 128], tp[tb][:, kk, :], Asc[:, k:k + 1], modF[:, k, bcol:bcol + 1], ALU.mult, ALU.add),
                                    reads=["tp%d" % tb, "A1", "Ac", "modF"], writes=["hT%d_%d" % (i, k)])
                    cut(20 + i)
                P.disabled = False
                if "hxT" in dbg_d:
                    P.barrier()
                    P.dma("sp", lambda e: e.dma_start(out=dbg_d["hxT"], in_=hxT[:]), writes=["dbg"])
                P.barrier()
                P.emit_block()
            if stop_after == "B":
                return nc

            w_in_v = w_in_d.rearrange("(k p) n -> p k n", p=128)

            with ExitStack() as st:
                wts = [sb(st, "wts%d" % i, [128, 16, 128], BF16) for i in range(4)]
                wrr = [0]
                QT = sb(st, "QT", [128, T], BF16)
                KT = sb(st, "KT", [128, T + LC], BF16)
                Ve = sb(st, "Ve", [128, 18, 128], BF16)
                Vo = sb(st, "Vo", [128, 15, 128], BF16)
                nab = sb(st, "nab", [128, 2, 7, 64])
                namask = sb(st, "namask", [128, 64])
                sw = [sb(st, "sw%d" % i, [128, 4, 64]) for i in range(2)]
                pT = [sb(st, "pT%d" % i, [128, 6, 64], BF16) for i in range(2)]
                rd = [sb(st, "rd%d" % i, [128, 64]) for i in range(2)]
                catT = [sb(st, "catT%d" % i, [128, T], BF16) for i in range(2)]
                pj = [ps(st, "pj%d" % i, [128, 512]) for i in range(2)]
                pv = [ps(st, "pv%d" % i, [128, 4, 128]) for i in range(2)]
                scp = [ps(st, "scp%d" % i, [128, 6, 64]) for i in range(2)]
                ndp = [ps(st, "ndp%d" % i, [128, 2, 64]) for i in range(2)]
                pjr = [0]
                P.dma("sp", lambda e: e.dma_start(out=namask[:], in_=namask_d), writes=["namask"])

                def load_w(col0):
                    wi = wrr[0] % 4
                    wrr[0] += 1
                    P.dma("pool", lambda e: e.dma_start(out=wts[wi][:], in_=w_in_v[:, :, col0:col0 + 128]), writes=["wts%d" % wi])
                    return wi

                def proj_fm(wi, srcT, srckey, n0, n, evac):
                    pi = pjr[0] % 2
                    pjr[0] += 1

                    def f(e):
                        ins = None
                        for k in range(16):
                            ins = e.matmul(pj[pi][:, 0:n], wts[wi][:, k, :], srcT[:, k, n0:n0 + n], start=(k == 0), stop=(k == 15))
                        return ins
                    P.op("pe", f, reads=["wts%d" % wi, srckey], writes=["pj%d" % pi])
                    evac(pj[pi][:, 0:n], "pj%d" % pi)

                pvr = [0]

                def proj_tm(wi, tiles, dst, dstkey):
                    for g0 in range(0, len(tiles), 4):
                        grp = tiles[g0:g0 + 4]
                        pi = pvr[0] % 2
                        pvr[0] += 1

                        def f(e, grp=grp, pi=pi):
                            ins = None
                            for gi, (srcT, srckey, tok0) in enumerate(grp):
                                for k in range(16):
                                    ins = e.matmul(pv[pi][:, gi, :], srcT[:, k, tok0:tok0 + 128], wts[wi][:, k, :], start=(k == 0), stop=(k == 15))
                            return ins
                        P.op("pe", f, reads=["wts%d" % wi] + [t[1] for t in grp], writes=["pv%d" % pi])
                        ng = len(grp)
                        P.op("dve", lambda e, pi=pi, g0=g0, ng=ng: e.tensor_copy(dst[:, g0:g0 + ng, :], pv[pi][:, 0:ng, :]), reads=["pv%d" % pi], writes=[dstkey])

                xkeys = "hxT"
                for h in range(NH if stop_after != "C2a" else 0):
                    ci = h % 2
                    wq = load_w(0 * 1024 + h * 128)
                    wk = load_w(1 * 1024 + h * 128)
                    wv = load_w(2 * 1024 + h * 128)
                    P.dma("sp", lambda e, h=h: e.dma_start(out=nab[:], in_=nag_d[h]), writes=["nab"])
                    for par in range(2):
                        for m in range(7):
                            P.op("pool", lambda e, par=par, m=m: e.tensor_tensor(nab[:, par, m, :], nab[:, par, m, :], namask[:], ALU.add), reads=["nab", "namask"], writes=["nab"])
                    for c4 in range(4):
                        proj_fm(wq, hxT, "hxT", c4 * 512, 512, lambda pap, pk, c4=c4: P.op(
                            "act", lambda e: e.activation(QT[:, c4 * 512:(c4 + 1) * 512], pap, AF.Copy, scale=DH ** -0.5), reads=[pk], writes=["QT"]))
                        proj_fm(wk, hxT, "hxT", c4 * 512, 512, lambda pap, pk, c4=c4: P.op(
                            "dve", lambda e: e.tensor_copy(KT[:, c4 * 512:(c4 + 1) * 512], pap), reads=[pk], writes=["KT"]))
                    proj_fm(wk, hcT, "hcT", 0, LC, lambda pap, pk: P.op(
                        "dve", lambda e: e.tensor_copy(KT[:, T:T + LC], pap), reads=[pk], writes=["KT"]))
                    proj_tm(wv, [(hxT, "hxT", i * 128) for i in range(16)] + [(hcT, "hcT", 0), (hcT, "hcT", 128)], Ve, "Ve")
                    proj_tm(wv, [(hxT, "hxT", 64 + i * 128) for i in range(15)], Vo, "Vo")
                    for r in range(32):
                        bi = r % 2
                        ks = min(max(r - 4, 0), 24)
                        d0 = ks - r + 7
                        par, m0 = d0 % 2, d0 // 2

                        def fqk(e, r=r, ks=ks, bi=bi):
                            ins = None
                            for j in range(6):
                                k0 = (ks + 2 * j) * 64 if j < 4 else T + (j - 4) * 128
                                ins = e.matmul(scp[bi][:, j, :], KT[:, k0:k0 + 128], QT[:, r * 64:(r + 1) * 64], start=True, stop=True)
                            return ins
                        P.op("pe", fqk, reads=["QT", "KT"], writes=["scp%d" % bi])
                        P.op("dve", lambda e, bi=bi, par=par, m0=m0: e.tensor_tensor(sw[bi][:], scp[bi][:, 0:4, :], nab[:, par, m0:m0 + 4, :], ALU.add),
                             reads=["scp%d" % bi, "nab"], writes=["sw%d" % bi])
                        P.op("act", lambda e, bi=bi: e.activation(pT[bi][:, 0:4, :], sw[bi][:], AF.Exp), reads=["sw%d" % bi], writes=["pTa%d" % bi])
                        P.op("act", lambda e, bi=bi: e.activation(pT[bi][:, 4:6, :], scp[bi][:, 4:6, :], AF.Exp), reads=["scp%d" % bi], writes=["pTb%d" % bi])

                        def fpv(e, r=r, ks=ks, bi=bi):
                            ins = None
                            for j in range(6):
                                if j < 4:
                                    kr_ = ks + 2 * j
                                    vt = Ve[:, kr_ // 2, :] if kr_ % 2 == 0 else Vo[:, (kr_ - 1) // 2, :]
                                else:
                                    vt = Ve[:, 16 + (j - 4), :]
                                ins = e.matmul(ndp[bi][:, 0, :], vt, pT[bi][:, j, :], start=(j == 0), stop=(j == 5))
                            for j in range(6):
                                ins = e.matmul(ndp[bi][:, 1, :], onesb[:], pT[bi][:, j, :], start=(j == 0), stop=(j == 5))
                            return ins
                        P.op("pe", fpv, reads=["Ve", "Vo", "pTa%d" % bi, "pTb%d" % bi, "onesb"], writes=["ndp%d" % bi])
                        P.op("dve", lambda e, bi=bi: e.reciprocal(rd[bi][:], ndp[bi][:, 1, :]), reads=["ndp%d" % bi], writes=["rd%d" % bi])
                        P.op("dve", lambda e, bi=bi, r=r, ci=ci: e.tensor_tensor(catT[ci][:, r * 64:(r + 1) * 64], ndp[bi][:, 0, :], rd[bi][:], ALU.mult),
                             reads=["ndp%d" % bi, "rd%d" % bi], writes=["catT%d" % ci])
                    P.dma("sp", lambda e, h=h, ci=ci: e.dma_start(out=cat_d[h], in_=catT[ci][:]), reads=["catT%d" % ci], writes=["cat_d%d" % h])
                    if h == 0 and "na0" in dbg_d:
                        P.dma("sp", lambda e, ci=ci: e.dma_start(out=dbg_d["na0"], in_=catT[ci][:]), reads=["catT%d" % ci], writes=["dbg"])
                    if stop_after == "C1a" and h == 0:
                        break
                P.barrier()
                P.emit_block()
            if stop_after in ("C1", "C1a"):
                return nc

            TE = T + LC
            NCK = TE // CH
            with ExitStack() as st:
                wts = [sb(st, "wth%d" % i, [128, 16, 128], BF16) for i in range(3)]
                wrr = [0]
                B0 = sb(st, "B0", [128, TE])
                B1 = sb(st, "B1", [128, TE])
                B2 = sb(st, "B2", [128, TE])
                B3 = sb(st, "B3", [128, TE])
                B5 = sb(st, "B5", [128, T])
                oacc = sb(st, "oacc", [128, T])
                tmpq = [sb(st, "tmpq%d" % i, [128, 512]) for i in range(2)]
                rC = sb(st, "rC", [128, T], BF16)
                rS = sb(st, "rS", [128, T], BF16)
                smask = sb(st, "smask", [128, TE], BF16)
                permf = sb(st, "permf", [128, 128])
                bdm = [sb(st, "bdm%d" % i, [128, 4, 128]) for i in range(2)]
                kp = sb(st, "kp", [128, TE], BF16)
                kdfm = sb(st, "kdfm", [128, TE], BF16)
                qp = sb(st, "qp", [128, T], BF16)
                Vh = sb(st, "Vh", [128, 18, 128], BF16)
                kdT = sb(st, "kdT", [128, 18, 128], BF16)
                AT = sb(st, "AT", [128, 16, 128], BF16)
                Sb = sb(st, "Sb", [128, 64, 128], BF16)
                S32 = sb(st, "S32", [128, 4, 128])
                tot = sb(st, "tot", [128, NCK])
                pj = [ps(st, "hpj%d" % i, [128, 512]) for i in range(2)]
                pv = [ps(st, "hpv%d" % i, [128, 4, 128]) for i in range(2)]
                pu = [ps(st, "hpu%d" % i, [128, 4, 128]) for i in range(4)]
                pjr = [0]
                pvr = [0]
                P.dma("pool", lambda e: e.dma_start(out=rC[:], in_=ropeC_d), writes=["rC"])
                P.dma("pool", lambda e: e.dma_start(out=rS[:], in_=ropeS_d), writes=["rS"])
                P.dma("sp", lambda e: e.dma_start(out=permf[:], in_=perm_d), writes=["permf"])
                P.dma("sp", lambda e: e.dma_start(out=bdm[0][:], in_=trif_d), writes=["bdm0"])
                P.dma("sp", lambda e: e.dma_start(out=bdm[1][:], in_=trib_d), writes=["bdm1"])
                smv = smask[:].rearrange("p (c s) -> p c s", s=CH)
                P.op("pool", lambda e: e.memset(smask[:], 1.0), writes=["smask"])
                P.op("pool", lambda e: e.memset(smv[:, :, 0:1], 0.0), writes=["smask"])
                cut(40)

                def v3(buf):
                    return buf[:].rearrange("p (c s) -> p c s", s=CH)

                def load_wh(col0):
                    wi = wrr[0] % 3
                    wrr[0] += 1
                    P.dma("pool", lambda e: e.dma_start(out=wts[wi][:], in_=w_in_v[:, :, col0:col0 + 128]), writes=["wth%d" % wi])
                    return wi

                def proj_fm(wi, srcT, srckey, n0, n, evac):
                    pi = pjr[0] % 2
                    pjr[0] += 1

                    def f(e):
                        ins = None
                        for k in range(16):
                            ins = e.matmul(pj[pi][:, 0:n], wts[wi][:, k, :], srcT[:, k, n0:n0 + n], start=(k == 0), stop=(k == 15))
                        return ins
                    P.op("pe", f, reads=["wth%d" % wi, srckey], writes=["hpj%d" % pi])
                    evac(pj[pi][:, 0:n], "hpj%d" % pi)

                def mm_f32(lhsT, lkey, rhs, rkey, evac):
                    pi = pjr[0] % 2
                    pjr[0] += 1
                    n = rhs.shape[1]
                    P.op("pe", lambda e: e.matmul(pj[pi][:, 0:n], lhsT, rhs, start=True, stop=True), reads=[lkey, rkey], writes=["hpj%d" % pi])
                    evac(pj[pi][:, 0:n], "hpj%d" % pi)

                for h in range(NH):
                    wqh = load_wh(3 * 1024 + h * 128)
                    wih = load_wh(6 * 1024 + h * 128)
                    tiles = [(hxT, "hxT", i * 128) for i in range(16)] + [(hcT, "hcT", 0), (hcT, "hcT", 128)]
                    for g0 in range(0, 18, 4):
                        grp = tiles[g0:g0 + 4]
                        pi = pvr[0] % 2
                        pvr[0] += 1

                        def f(e, grp=grp, pi=pi, wih=wih):
                            ins = None
                            for gi, (srcT, srckey, tok0) in enumerate(grp):
                                for k in range(16):
                                    ins = e.matmul(pv[pi][:, gi, :], srcT[:, k, tok0:tok0 + 128], wts[wih][:, k, :], start=(k == 0), stop=(k == 15))
                            return ins
                        P.op("pe", f, reads=["wth%d" % wih, "hxT", "hcT"], writes=["hpv%d" % pi])
                        ng = len(grp)
                        P.op("act", lambda e, pi=pi, g0=g0, ng=ng: e.activation(Vh[:, g0:g0 + ng, :], pv[pi][:, 0:ng, :], AF.Copy), reads=["hpv%d" % pi], writes=["Vh"])
                    cut(41)
                    for c4 in range(4):
                        rng = slice(c4 * 512, (c4 + 1) * 512)
                        proj_fm(wqh, hxT, "hxT", c4 * 512, 512, lambda pap, pk, rng=rng, c4=c4: P.op(
                            "dve", lambda e: e.tensor_copy(B5[:, rng], pap), reads=[pk], writes=["B5_%d" % c4]))
                    for c4 in range(4):
                        rng = slice(c4 * 512, (c4 + 1) * 512)
                        tq = tmpq[c4 % 2]
                        tk = "tmpq%d" % (c4 % 2)

                        def evq(pap, pk, rng=rng, c4=c4, tq=tq, tk=tk):
                            P.op("dve", lambda e: e.tensor_tensor(tq[:], pap, rS[:, rng], ALU.mult), reads=[pk, "rS"], writes=[tk])
                            P.op("pool", lambda e: e.tensor_tensor(B5[:, rng], B5[:, rng], rC[:, rng], ALU.mult), reads=["B5_%d" % c4, "rC"], writes=["B5_%d" % c4])
                            P.op("pool", lambda e: e.tensor_tensor(B5[:, rng], B5[:, rng], tq[:], ALU.add), reads=["B5_%d" % c4, tk], writes=["B5_%d" % c4])
                        mm_f32(permf[:], "permf", B5[:, rng], "B5_%d" % c4, evq)
                    B5k = ["B5_%d" % c for c in range(4)]
                    cut(42)

                    for dd in range(2):
                        wf = load_wh((4 + dd) * 1024 + h * 128)
                        lc = dd * 8 + h
                        lb_ap, oml_ap, noml_ap = lbF[:, lc:lc + 1], omlF[:, lc:lc + 1], nomlF[:, lc:lc + 1]
                        for c4 in range(4):
                            rng = slice(c4 * 512, (c4 + 1) * 512)
                            proj_fm(wf, hxT, "hxT", c4 * 512, 512, lambda pap, pk, rng=rng: P.op(
                                "act", lambda e: e.activation(B0[:, rng], pap, AF.Exp, scale=-1.0), reads=[pk], writes=["B0"]))
                        proj_fm(wf, hcT, "hcT", 0, LC, lambda pap, pk: P.op(
                            "act", lambda e: e.activation(B0[:, T:TE], pap, AF.Exp, scale=-1.0), reads=[pk], writes=["B0"]))
                        P.op("dve", lambda e: e.tensor_scalar(B0[:], B0[:], 1.0, None, ALU.add), reads=["B0"], writes=["B0"])
                        P.op("dve", lambda e: e.reciprocal(B0[:], B0[:]), reads=["B0"], writes=["B0"])
                        P.op("act", lambda e, oml_ap=oml_ap, lb_ap=lb_ap: e.activation(B1[:], B0[:], AF.Ln, scale=oml_ap, bias=lb_ap), reads=["B0", "lbF", "omlF"], writes=["B1"])
                        P.op("pool", lambda e, oml_ap=oml_ap, noml_ap=noml_ap: e.tensor_scalar(B0[:], B0[:], noml_ap, oml_ap, ALU.mult, ALU.add), reads=["B0", "omlF", "nomlF"], writes=["B0"])
                        cut(43)
                        P.op("dve", lambda e: e.tensor_tensor_scan(B2[:], smask[:], B1[:], 0.0, ALU.mult, ALU.add), reads=["smask", "B1"], writes=["B2"])
                        cut(44)
                        if dd == 1:
                            P.op("dve", lambda e: e.tensor_tensor(B2[:], B2[:], B1[:], ALU.subtract), reads=["B2", "B1"], writes=["B2"])
                            P.op("dve", lambda e: e.tensor_tensor(tot[:], v3(B2)[:, :, CH - 1], v3(B1)[:, :, CH - 1], ALU.add), reads=["B2", "B1"], writes=["tot"])
                            P.op("dve", lambda e: e.tensor_tensor(v3(B2), tot[:].unsqueeze(2).to_broadcast([128, NCK, CH]), v3(B2), ALU.subtract), reads=["B2", "tot"], writes=["B2"])
                        P.op("act", lambda e: e.activation(B1[:], B2[:], AF.Exp), reads=["B2"], writes=["B1"])
                        P.op("act", lambda e: e.activation(B2[:], B2[:], AF.Exp, scale=-1.0), reads=["B2"], writes=["B2"])
                        cut(45)
                        for c4 in range(4):
                            rng = slice(c4 * 512, (c4 + 1) * 512)
                            tq = tmpq[c4 % 2]
                            tk = "tmpq%d" % (c4 % 2)

                            def evk(pap, pk, rng=rng, tq=tq, tk=tk):
                                P.op("dve", lambda e: e.tensor_tensor(tq[:], pap, rS[:, rng], ALU.mult), reads=[pk, "rS"], writes=[tk])
                                P.op("pool", lambda e: e.tensor_tensor(B3[:, rng], B0[:, rng], rC[:, rng], ALU.mult), reads=["B0", "rC"], writes=["B3"])
                                P.op("pool", lambda e: e.tensor_tensor(B3[:, rng], B3[:, rng], tq[:], ALU.add), reads=["B3", tk], writes=["B3"])
                            mm_f32(permf[:], "permf", B0[:, rng], "B0", evk)
                        P.op("pool", lambda e: e.tensor_copy(B3[:, T:TE], B0[:, T:TE]), reads=["B0"], writes=["B3"])
                        cut(46)
                        P.op("pool", lambda e: e.tensor_tensor(B3[:], B3[:], B2[:], ALU.mult), reads=["B3", "B2"], writes=["B3"])
                        P.op("act", lambda e: e.activation(kp[:], B3[:], AF.Copy), reads=["B3"], writes=["kp"])
                        di = CH - 1 if dd == 0 else 0
                        P.op("dve", lambda e, di=di: e.tensor_tensor(v3(kdfm), v3(B3), v3(B1)[:, :, di:di + 1].to_broadcast([128, NCK, CH]), ALU.mult), reads=["B3", "B1"], writes=["kdfm"])
                        P.op("dve", lambda e: e.tensor_tensor(qp[:], B5[:], B1[:, 0:T], ALU.mult), reads=B5k + ["B1"], writes=["qp"])
                        cut(47)
                        for g0 in range(0, 18, 4):
                            ng = min(4, 18 - g0)

                            pi = pvr[0] % 2
                            pvr[0] += 1
                            ptr = pv[pi][:].rearrange("p a b -> p (a b)").bitcast(BF16)[:, 0:512].rearrange("p (a b) -> p a b", b=128)

                            def ftr(e, g0=g0, ng=ng, ptr=ptr):
                                ins = None
                                for gi in range(ng):
                                    g = g0 + gi
                                    ins = e.transpose(ptr[:, gi, :], kdfm[:, g * 128:(g + 1) * 128], identb[:])
                                return ins
                            P.op("pe", ftr, reads=["kdfm", "identb"], writes=["hpv%d" % pi])
                            P.op("act", lambda e, g0=g0, ng=ng, ptr=ptr: e.activation(kdT[:, g0:g0 + ng, :], ptr[:, 0:ng, :], AF.Copy), reads=["hpv%d" % pi], writes=["kdT"])
                        cut(48)
                        for g0 in range(0, 16, 4):
                            pi = pvr[0] % 2
                            pvr[0] += 1

                            def fa(e, g0=g0, pi=pi):
                                ins = None
                                for gi in range(4):
                                    g = g0 + gi
                                    ins = e.matmul(pv[pi][:, gi, :], kp[:, g * 128:(g + 1) * 128], qp[:, g * 128:(g + 1) * 128], start=True, stop=True)
                                return ins
                            P.op("pe", fa, reads=["kp", "qp"], writes=["hpv%d" % pi])
                            P.op("dve", lambda e, g0=g0, pi=pi, dd=dd: e.tensor_tensor(AT[:, g0:g0 + 4, :], pv[pi][:], bdm[dd][:], ALU.mult), reads=["hpv%d" % pi, "bdm%d" % dd], writes=["AT"])
                        cut(49)
                        order = (list(range(64, 72)) + list(range(0, 64))) if dd == 0 else (list(range(71, 63, -1)) + list(range(63, -1, -1)))
                        P.op("pool", lambda e: e.memset(S32[:, 0, :], 0.0), writes=["S32_0"])
                        first_x = order[8]
                        for n0 in range(0, NCK, 4):
                            cs = order[n0:n0 + 4]
                            for j, c in enumerate(cs):
                                n = n0 + j
                                ui = c % 4
                                sl = (c // 4) % 4
                                pb = (c % 4) * 32
                                if os.environ.get("KSKIP") != "umm":
                                  P.op("pe", lambda e, c=c, ui=ui, sl=sl, pb=pb: e.matmul(pu[ui][:, sl, :], kdT[pb:pb + 32, c // 4, :], Vh[pb:pb + 32, c // 4, :], start=True, stop=True, tile_position=(pb, 0)),
                                       reads=["kdT", "Vh"], writes=["hpu%d_%d" % (ui, sl)])
                                if os.environ.get("KSKIP") == "chain":
                                    continue
                                if n >= 8 and os.environ.get("KSKIP") != "actcopy":
                                    P.op("act", lambda e, n=n, c=c: e.activation(Sb[:, c, :], S32[:, n % 4, :], AF.Copy), reads=["S32_%d" % (n % 4)], writes=["Sb"])
                                if n == NCK - 1:
                                    break
                                col = c * CH + di
                                P.op("dve", lambda e, n=n, col=col: e.tensor_scalar(S32[:, (n + 1) % 4, :], S32[:, n % 4, :], B1[:, col:col + 1], None, ALU.mult),
                                     reads=["S32_%d" % (n % 4), "B1"], writes=["S32_%d" % ((n + 1) % 4)])
                                P.op("dve", lambda e, n=n, sl=sl, ui=ui: e.tensor_tensor(S32[:, (n + 1) % 4, :], pu[ui][:, sl, :], S32[:, (n + 1) % 4, :], ALU.add),
                                     reads=["S32_%d" % ((n + 1) % 4), "hpu%d_%d" % (ui, sl)], writes=["S32_%d" % ((n + 1) % 4)])
                        cut(50)
                        for g0 in range(0, 16, 4):
                            pi = pvr[0] % 2
                            pvr[0] += 1
                            po = pv[pi]

                            def fo(e, g0=g0, po=po):
                                ins = None
                                for gi in range(4):
                                    g = g0 + gi
                                    ins = e.matmul(po[:, gi, :], Vh[:, g, :], AT[:, g, :], start=True, stop=False)
                                    for cc in range(4):
                                        c = 4 * g + cc
                                        ins = e.matmul(po[:, gi, cc * CH:(cc + 1) * CH], Sb[:, c, :], qp[:, c * CH:(c + 1) * CH], start=False, stop=(cc == 3))
                                return ins
                            P.op("pe", fo, reads=["Vh", "AT", "Sb", "qp"], writes=["hpv%d" % pi])
                            osl = oacc[:, g0 * 128:(g0 + 4) * 128].rearrange("p (g t) -> p g t", t=128)
                            if dd == 0:
                                P.op("dve", lambda e, osl=osl, po=po: e.tensor_copy(osl, po[:]), reads=["hpv%d" % pi], writes=["oacc"])
                            else:
                                P.op("dve", lambda e, osl=osl, po=po: e.tensor_tensor(osl, po[:], osl, ALU.add), reads=["hpv%d" % pi, "oacc"], writes=["oacc"])
                        if h == 0 and dd == 0 and "of0" in dbg_d:
                            P.dma("sp", lambda e: e.dma_start(out=dbg_d["of0"], in_=oacc[:]), reads=["oacc"], writes=["dbg"])
                    cut(51)
                    wgh = load_wh(7 * 1024 + h * 128)
                    cut(59)
                    for c4 in range(4):
                        rng = slice(c4 * 512, (c4 + 1) * 512)

                        def evg(pap, pk, rng=rng):
                            if os.environ.get("KSKIP") != "gdve":
                                P.op("dve", lambda e: e.tensor_copy(B0[:, rng], pap), reads=[pk], writes=["B0"])
                            if os.environ.get("KSKIP") != "gact":
                                P.op("act", lambda e: e.activation(B2[:, rng], B0[:, rng], AF.Exp, scale=-1.0), reads=["B0"], writes=["B2"])
                        proj_fm(wgh, hxT, "hxT", c4 * 512, 512, evg)
                        cut(60 + c4)
                    P.op("dve", lambda e: e.tensor_scalar(B2[:, 0:T], B2[:, 0:T], 1.0, None, ALU.add), reads=["B2"], writes=["B2"])
                    P.op("dve", lambda e: e.reciprocal(B2[:, 0:T], B2[:, 0:T]), reads=["B2"], writes=["B2"])
                    P.op("pool", lambda e: e.tensor_tensor(B0[:, 0:T], B0[:, 0:T], B2[:, 0:T], ALU.mult), reads=["B0", "B2"], writes=["B0"])
                    cut(52)
                    P.op("act", lambda e: e.activation(B3[:, 0:T], oacc[:], AF.Square), reads=["oacc"], writes=["B3"])
                    cut(53)
                    for c4 in range(4):
                        rng = slice(c4 * 512, (c4 + 1) * 512)

                        def evn(pap, pk, rng=rng):
                            P.op("act", lambda e: e.activation(B1[:, rng], pap, AF.Ln, scale=1.0 / DH, bias=EPS), reads=[pk], writes=["B1"])
                        mm_f32(onesf[:], "onesf", B3[:, rng], "B3", evn)
                    P.op("act", lambda e: e.activation(B1[:, 0:T], B1[:, 0:T], AF.Exp, scale=-0.5), reads=["B1"], writes=["B1"])
                    cut(54)
                    P.op("dve", lambda e: e.tensor_tensor(B3[:, 0:T], oacc[:], B1[:, 0:T], ALU.mult), reads=["oacc", "B1", "B3"], writes=["B3"])
                    P.op("pool", lambda e: e.tensor_scalar(B0[:, 0:T], B0[:, 0:T], hgF[:, 0:1], None, ALU.mult), reads=["B0", "hgF"], writes=["B0"])
                    P.op("dve", lambda e: e.tensor_tensor(qp[:], B3[:, 0:T], B0[:, 0:T], ALU.mult), reads=["B3", "B0"], writes=["qp"])
                    P.dma("sp", lambda e, h=h: e.dma_start(out=cat_d[8 + h], in_=qp[:]), reads=["qp"], writes=["cat_d%d" % (8 + h)])
                    if h == 0 and "hg0" in dbg_d:
                        P.dma("sp", lambda e: e.dma_start(out=dbg_d["hg0"], in_=qp[:]), reads=["qp"], writes=["dbg"])
                    if stop_after == "C2a" and h == 0:
                        break
                P.disabled = False
                P.barrier()
                P.emit_block()
            if stop_after in ("C2", "C2a"):
                return nc

        NSLOT = NE * CAP
        with ExitStack() as stR:
            idx = sb(stR, "idx", [128, 16, 4], I32)
            gk = sb(stR, "gk", [128, 16, 4])
            with ExitStack() as st:
                wo = sb(st, "wo", [128, 16, D], BF16)
                hx2b = sb(st, "hx2b", [128, 16, D], BF16)
                g1b = sb(st, "g1b", [128, D])
                A2b = sb(st, "A2b", [128, D])
                B2b = sb(st, "B2b", [128, D])
                xt = sb(st, "xtD", [128, D])
                x1t = sb(st, "x1t", [128, D])
                xn = sb(st, "xnD", [128, D])
                ct = sb(st, "ct", [128, 16, 128], BF16)
                junk = sb(st, "junkD", [128, D], BF16)
                h2T = sb(st, "h2T", [128, 16, 128])
                wr = sb(st, "wr", [128, 16, NE])
                brb = sb(st, "brb", [128, NE])
                iotaC = sb(st, "iotaC", [128, NE])
                lstr = sb(st, "lstr", [128, 128])
                maskall = sb(st, "maskall", [128, 16, NE])
                lg = sb(st, "lg", [128, NE])
                mx8 = sb(st, "mx8", [128, 8])
                sm = sb(st, "sm", [128, 8])
                ex = sb(st, "ex", [128, NE])
                gfull = sb(st, "gfull", [128, NE])
                slotf = sb(st, "slotf", [128, NE])
                oh = sb(st, "oh", [128, NE])
                t32 = sb(st, "t32", [128, NE])
                slf = sb(st, "slf", [128, 16, 4])
                pjD = [ps(st, "pjD%d" % i, [128, 512]) for i in range(2)]
                tpD = [ps(st, "tpD%d" % i, [128, 4, 128]) for i in range(2)]
                plg = ps(st, "plg", [128, NE])
                ppos = ps(st, "ppos", [128, NE])

                wov = w_out_d.rearrange("(k p) n -> p k n", p=128)
                for dc in range(4):
                    P.dma("pool", lambda e, dc=dc: e.dma_start(out=wo[:, :, dc * 512:(dc + 1) * 512], in_=wov[:, :, dc * 512:(dc + 1) * 512]), writes=["wo"])
                P.dma("sp", lambda e: e.dma_start(out=g1b[:], in_=bc_d[0:1, :].broadcast_to([128, D])), writes=["g1b"])
                P.dma("sp", lambda e: e.dma_start(out=A2b[:], in_=bc_d[1:2, :].broadcast_to([128, D])), writes=["A2b"])
                P.dma("sp", lambda e: e.dma_start(out=B2b[:], in_=bc_d[2:3, :].broadcast_to([128, D])), writes=["B2b"])
                P.dma("sp", lambda e: e.dma_start(out=wr[:], in_=w_r_d.rearrange("(k p) n -> p k n", p=128)), writes=["wr"])
                P.dma("sp", lambda e: e.dma_start(out=brb[:], in_=b_r_d.broadcast_to([128, NE])), writes=["brb"])
                P.dma("sp", lambda e: e.dma_start(out=iotaC[:], in_=iotae_d), writes=["iotaC"])
                P.dma("sp", lambda e: e.dma_start(out=lstr[:], in_=lstrict_d), writes=["lstr"])
                P.op("dve", lambda e: e.tensor_scalar(iotaC[:], iotaC[:], float(CAP), None, ALU.mult), reads=["iotaC"], writes=["iotaC"])
                P.op("pool", lambda e: e.memset(junk[:], 0.0), writes=["junkD"])
                xs_v = xs_d[0:NSLOT + 128, :].rearrange("(j p) d -> j p d", p=128)
                for jj in range(NSLOT // 128 + 1):
                    P.dma("sp", lambda e, jj=jj: e.dma_start(out=xs_v[jj], in_=junk[:]), reads=["junkD"], writes=["xs_d"])

                catv = cat_d.rearrange("k p t -> p k t")
                for i in range(16):
                    tsl = slice(i * 128, (i + 1) * 128)
                    P.dma("sp", lambda e, tsl=tsl: e.dma_start(out=ct[:], in_=catv[:, :, tsl]), writes=["ct"])
                    P.dma("sp", lambda e, tsl=tsl: e.dma_start(out=xt[:], in_=x_d[tsl, :]), writes=["xtD"])
                    if "cat" in dbg_d:
                        P.dma("sp", lambda e, i=i: e.dma_start(out=dbg_d["cat"][i], in_=ct[:]), reads=["ct"], writes=["dbg"])
                    for dc in range(4):
                        pi = dc % 2
                        rng = slice(dc * 512, (dc + 1) * 512)

                        def fo(e, pi=pi, rng=rng):
                            ins = None
                            for k in range(16):
                                ins = e.matmul(pjD[pi][:], ct[:, k, :], wo[:, k, rng], start=(k == 0), stop=(k == 15))
                            return ins
                        P.op("pe", fo, reads=["ct", "wo"], writes=["pjD%d" % pi])
                        P.op("dve", lambda e, pi=pi, rng=rng: e.tensor_tensor(x1t[:, rng], pjD[pi][:], g1b[:, rng], ALU.mult), reads=["pjD%d" % pi, "g1b"], writes=["x1t"])
                    P.op("pool", lambda e: e.tensor_tensor(x1t[:], x1t[:], xt[:], ALU.add), reads=["x1t", "xtD"], writes=["x1t"])
                    P.dma("sp", lambda e, tsl=tsl: e.dma_start(out=x1_d[tsl, :], in_=x1t[:]), reads=["x1t"], writes=["x1_d"])
                    if i == 0 and "x1" in dbg_d:
                        P.dma("sp", lambda e: e.dma_start(out=dbg_d["x1"], in_=x1t[:]), reads=["x1t"], writes=["dbg"])
                    P.op("act", lambda e: e.activation(junk[:], x1t[:], AF.Square, accum_out=sm[:, 0:1]), reads=["x1t"], writes=["junkD", "sm0"])
                    P.op("act", lambda e: e.activation(sm[:, 0:1], sm[:, 0:1], AF.Ln, scale=1.0 / D, bias=EPS), reads=["sm0"], writes=["sm0"])
                    P.op("act", lambda e: e.activation(sm[:, 0:1], sm[:, 0:1], AF.Exp, scale=-0.5), reads=["sm0"], writes=["sm0"])
                    P.op("dve", lambda e: e.tensor_scalar(xn[:], x1t[:], sm[:, 0:1], None, ALU.mult), reads=["x1t", "sm0"], writes=["xnD"])
                    P.op("dve", lambda e: e.tensor_tensor(xn[:], xn[:], A2b[:], ALU.mult), reads=["xnD", "A2b"], writes=["xnD"])
                    P.op("pool", lambda e: e.tensor_tensor(xn[:], xn[:], B2b[:], ALU.add), reads=["xnD", "B2b"], writes=["xnD"])
                    P.op("act", lambda e, i=i: e.activation(hx2b[:, i, :], xn[:], AF.Copy), reads=["xnD"], writes=["hx2b%d" % i])
                    for kg in range(4):
                        tb = kg % 2

                        def trD(e, kg=kg, tb=tb):
                            ins = None
                            for kk in range(4):
                                k = kg * 4 + kk
                                ins = e.transpose(tpD[tb][:, kk, :], xn[:, k * 128:(k + 1) * 128], identf[:])
                            return ins
                        P.op("pe", trD, reads=["xnD", "identf"], writes=["tpD%d" % tb])
                        P.op("dve", lambda e, kg=kg, tb=tb: e.tensor_copy(h2T[:, kg * 4:(kg + 1) * 4, :], tpD[tb][:]), reads=["tpD%d" % tb], writes=["h2T"])

                    def flg(e):
                        ins = None
                        for k in range(16):
                            ins = e.matmul(plg[:], h2T[:, k, :], wr[:, k, :], start=(k == 0), stop=(k == 15))
                        return ins
                    P.op("pe", flg, reads=["h2T", "wr"], writes=["plg"])
                    P.op("dve", lambda e: e.tensor_tensor(lg[:], plg[:], brb[:], ALU.add), reads=["plg", "brb"], writes=["lg"])
                    if i == 0 and "lg" in dbg_d:
                        P.dma("sp", lambda e: e.dma_start(out=dbg_d["lg"], in_=lg[:]), reads=["lg"], writes=["dbg"])
                    P.op("dve", lambda e: e.max(out=mx8[:], in_=lg[:]), reads=["lg"], writes=["mx8"])
                    P.op("dve", lambda e, i=i: e.tensor_scalar(maskall[:, i, :], lg[:], mx8[:, 3:4], None, ALU.is_ge), reads=["lg", "mx8"], writes=["mask%d" % i])
                    P.op("dve", lambda e: e.tensor_scalar(sm[:, 1:2], mx8[:, 0:1], -1.0, None, ALU.mult), reads=["mx8"], writes=["sm1"])
                    P.op("act", lambda e: e.activation(ex[:], lg[:], AF.Exp, bias=sm[:, 1:2]), reads=["lg", "sm1"], writes=["ex"])
                    P.op("dve", lambda e, i=i: e.tensor_tensor(ex[:], ex[:], maskall[:, i, :], ALU.mult), reads=["ex", "mask%d" % i], writes=["ex"])
                    P.op("dve", lambda e: e.tensor_reduce(sm[:, 2:3], ex[:], AX.X, ALU.add), reads=["ex"], writes=["sm2"])
                    P.op("dve", lambda e: e.reciprocal(sm[:, 2:3], sm[:, 2:3]), reads=["sm2"], writes=["sm2"])
                    P.op("dve", lambda e: e.tensor_scalar(gfull[:], ex[:], sm[:, 2:3], None, ALU.mult), reads=["ex", "sm2"], writes=["gfull"])

                    def fpos(e, i=i):
                        ins = e.matmul(ppos[:], lstr[:], maskall[:, i, :], start=True, stop=(i == 0))
                        for j in range(i):
                            ins = e.matmul(ppos[:], onesf[:], maskall[:, j, :], start=False, stop=(j == i - 1))
                        return ins
                    P.op("pe", fpos, reads=["lstr", "onesf"] + ["mask%d" % j for j in range(i + 1)], writes=["ppos"])
                    P.op("dve", lambda e: e.tensor_scalar(slotf[:], ppos[:], float(CAP), None, ALU.min), reads=["ppos"], writes=["slotf"])
                    P.op("dve", lambda e: e.tensor_tensor(slotf[:], slotf[:], iotaC[:], ALU.add), reads=["slotf", "iotaC"], writes=["slotf"])
                    for k in range(4):
                        P.op("dve", lambda e, k=k: e.tensor_scalar(oh[:], lg[:], mx8[:, k:k + 1], None, ALU.is_equal), reads=["lg", "mx8"], writes=["oh"])
                        P.op("dve", lambda e: e.tensor_tensor(t32[:], oh[:], slotf[:], ALU.mult), reads=["oh", "slotf"], writes=["t32"])
                        P.op("dve", lambda e, i=i, k=k: e.tensor_reduce(slf[:, i, k:k + 1], t32[:], AX.X, ALU.add), reads=["t32"], writes=["slf"])
                        P.op("dve", lambda e: e.tensor_tensor(t32[:], oh[:], gfull[:], ALU.mult), reads=["oh", "gfull", "slf"], writes=["t32"])
                        P.op("dve", lambda e, i=i, k=k: e.tensor_reduce(gk[:, i, k:k + 1], t32[:], AX.X, ALU.add), reads=["t32"], writes=["gk"])
                    P.op("dve", lambda e, i=i: e.tensor_copy(idx[:, i, :], slf[:, i, :]), reads=["slf"], writes=["idx"])
                    for k in range(4):
                        P.dma("pool", lambda e, i=i, k=k: e.indirect_dma_start(
                            out=xs_d, out_offset=bass.IndirectOffsetOnAxis(ap=idx[:, i, k:k + 1], axis=0), in_=hx2b[:, i, :], in_offset=None),
                            reads=["idx", "hx2b%d" % i], writes=["xs_d"])
                if "idx" in dbg_d:
                    P.dma("sp", lambda e: e.dma_start(out=dbg_d["idx"], in_=idx[:]), reads=["idx"], writes=["dbg"])
                    if "gk" in dbg_d:
                        P.dma("sp", lambda e: e.dma_start(out=dbg_d["gk"], in_=gk[:]), reads=["gk"], writes=["dbg"])
                P.barrier()
                P.emit_block()
            if stop_after == "E":
                return nc

            NJ = CAP // 128
            with ExitStack() as st:
                bguF = sb(st, "bguF", [128, 32, NE])
                pg = ps(st, "pg", [128, 1024])
                with ExitStack() as st2:
                    bgr = sb(st2, "bgr", [NE, 2 * D])
                    P.dma("sp", lambda e: e.dma_start(out=bgr[:], in_=b_gu_d), writes=["bgr"])
                    pgv = pg[:].rearrange("p (c e) -> p c e", e=NE)

                    def trb(e):
                        ins = None
                        for c in range(32):
                            ins = e.transpose(pgv[:, c, :], bgr[0:NE, c * 128:(c + 1) * 128], identf[0:NE, 0:NE])
                        return ins
                    P.op("pe", trb, reads=["bgr", "identf"], writes=["pg"])
                    P.op("dve", lambda e: e.tensor_copy(bguF[:], pgv), reads=["pg"], writes=["bguF"])
                    P.barrier()
                    P.emit_block()
                xrows2 = [sb(st, "xrows%d" % i, [128, NJ, D], BF16) for i in range(2)]
                XT = sb(st, "XT", [128, 16, CAP], BF16)
                actT = sb(st, "actT", [128, 16, CAP], BF16)
                wg = [sb(st, "wg%d" % i, [128, 16, 256], BF16) for i in range(2)]
                wl = [sb(st, "wl%d" % i, [128, 16, 256], BF16) for i in range(2)]
                wd = [sb(st, "wd%d" % i, [128, 16, 512], BF16) for i in range(2)]
                yout = [sb(st, "yout%d" % i, [128, NJ, 512]) for i in range(2)]
                bdn = sb(st, "bdn", [128, D])
                a_t = sb(st, "a_t", [128, CAP])
                s_t = sb(st, "s_t", [128, CAP], BF16)
                l_t = sb(st, "l_t", [128, CAP])
                ptx = [ps(st, "ptx%d" % i, [128, 4, 128], BF16) for i in range(2)]
                pl = ps(st, "pl", [128, 1024])
                pd = [ps(st, "pd%d" % i, [128, 512]) for i in range(2)]
                txr = [0]
                for ex_ in range(NE):
                    r0 = ex_ * CAP
                    xrows = xrows2[ex_ % 2]
                    xkey = "xrows%d" % (ex_ % 2)
                    if ex_ == 0:
                        P.dma("sp", lambda e, r0=r0, xrows=xrows: e.dma_start(out=xrows[:], in_=xs_d[r0:r0 + CAP, :].rearrange("(j p) d -> p j d", p=128)), reads=["xs_d"], writes=[xkey])
                    P.dma("sp", lambda e, ex_=ex_: e.dma_start(out=bdn[:], in_=b_dn_d[ex_:ex_ + 1, :].broadcast_to([128, D])), writes=["bdn"])
                    for j in range(NJ):
                        for kg in range(4):
                            tb = txr[0] % 2
                            txr[0] += 1

                            def trx(e, j=j, kg=kg, tb=tb, xrows=xrows):
                                ins = None
                                for kk in range(4):
                                    k = kg * 4 + kk
                                    ins = e.transpose(ptx[tb][:, kk, :], xrows[:, j, k * 128:(k + 1) * 128], identb[:])
                                return ins
                            P.op("pe", trx, reads=[xkey, "identb"], writes=["ptx%d" % tb])
                            dst = XT[:, kg * 4:(kg + 1) * 4, j * 128:(j + 1) * 128]
                            if txr[0] % 2 == 0:
                                P.op("act", lambda e, dst=dst, tb=tb: e.activation(dst, ptx[tb][:], AF.Copy), reads=["ptx%d" % tb], writes=["XT"])
                            else:
                                P.op("dve", lambda e, dst=dst, tb=tb: e.tensor_copy(dst, ptx[tb][:]), reads=["ptx%d" % tb], writes=["XT"])
                    if ex_ + 1 < NE:
                        r1 = (ex_ + 1) * CAP
                        xn_ = xrows2[(ex_ + 1) % 2]
                        P.dma("sp", lambda e, r1=r1, xn_=xn_: e.dma_start(out=xn_[:], in_=xs_d[r1:r1 + CAP, :].rearrange("(j p) d -> p j d", p=128)), reads=["xs_d"], writes=["xrows%d" % ((ex_ + 1) % 2)])
                    wguv = w_gu_d[ex_].rearrange("(k p) n -> p k n", p=128)
                    for G in range(8):
                        bi = G % 2
                        P.dma("pool", lambda e, G=G, bi=bi, wguv=wguv: e.dma_start(out=wg[bi][:], in_=wguv[:, :, G * 256:(G + 1) * 256]), writes=["wg%d" % bi])
                        P.dma("pool", lambda e, G=G, bi=bi, wguv=wguv: e.dma_start(out=wl[bi][:], in_=wguv[:, :, D + G * 256:D + (G + 1) * 256]), writes=["wl%d" % bi])
                        for cc in range(2):
                            ffc = 2 * G + cc

                            def fgu(e, wt, pt, cc=cc):
                                ins = None
                                for k in range(16):
                                    ins = e.matmul(pt[:, 0:512], wt[:, k, cc * 128:(cc + 1) * 128], XT[:, k, 0:512], start=(k == 0), stop=(k == 15))
                                for k in range(16):
                                    ins = e.matmul(pt[:, 512:CAP], wt[:, k, cc * 128:(cc + 1) * 128], XT[:, k, 512:CAP], start=(k == 0), stop=(k == 15))
                                return ins
                            P.op("pe", lambda e, bi=bi, fgu=fgu: fgu(e, wg[bi], pg), reads=["wg%d" % bi, "XT"], writes=["pg"])
                            P.op("pe", lambda e, bi=bi, fgu=fgu: fgu(e, wl[bi], pl), reads=["wl%d" % bi, "XT"], writes=["pl"])
                            P.op("dve", lambda e, ffc=ffc, ex_=ex_: e.tensor_scalar(a_t[:], pg[:, 0:CAP], bguF[:, ffc, ex_:ex_ + 1], 7.0, ALU.add, ALU.min), reads=["pg", "bguF"], writes=["a_t"])
                            P.op("act", lambda e: e.activation(s_t[:], a_t[:], AF.Sigmoid, scale=1.702), reads=["a_t"], writes=["s_t"])
                            P.op("act", lambda e, ffc=ffc, ex_=ex_: e.activation(l_t[:], pl[:, 0:CAP], AF.Identity, bias=bguF[:, 16 + ffc, ex_:ex_ + 1]), reads=["pl", "bguF"], writes=["l_t"])
                            P.op("dve", lambda e: e.tensor_scalar(l_t[:], l_t[:], -7.0, 7.0, ALU.max, ALU.min), reads=["l_t"], writes=["l_t"])
                            P.op("dve", lambda e: e.tensor_tensor(a_t[:], a_t[:], s_t[:], ALU.mult), reads=["a_t", "s_t"], writes=["a_t"])
                            P.op("dve", lambda e, ffc=ffc: e.scalar_tensor_tensor(actT[:, ffc, :], l_t[:], 1.0, a_t[:], ALU.add, ALU.mult), reads=["l_t", "a_t"], writes=["actT"])
                    wdnv = w_dn_d[ex_].rearrange("(k p) n -> p k n", p=128)
                    for dc in range(4):
                        bi = dc % 2
                        rng = slice(dc * 512, (dc + 1) * 512)
                        P.dma("pool", lambda e, bi=bi, rng=rng, wdnv=wdnv: e.dma_start(out=wd[bi][:], in_=wdnv[:, :, rng]), writes=["wd%d" % bi])
                        for j in range(NJ):
                            pi = j % 2

                            def fdn(e, j=j, pi=pi, bi=bi):
                                ins = None
                                for k in range(16):
                                    ins = e.matmul(pd[pi][:], actT[:, k, j * 128:(j + 1) * 128], wd[bi][:, k, :], start=(k == 0), stop=(k == 15))
                                return ins
                            P.op("pe", fdn, reads=["actT", "wd%d" % bi], writes=["pd%d" % pi])
                            P.op("dve", lambda e, j=j, pi=pi, bi=bi, rng=rng: e.tensor_tensor(yout[bi][:, j, :], pd[pi][:], bdn[:, rng], ALU.add), reads=["pd%d" % pi, "bdn"], writes=["yout%d" % bi])
                        P.dma("sp", lambda e, bi=bi, rng=rng, r0=r0: e.dma_start(out=ys_d[r0:r0 + CAP, rng].rearrange("(j p) d -> p j d", p=128), in_=yout[bi][:]), reads=["yout%d" % bi], writes=["ys_d"])
                        if ex_ == 0 and "y0" in dbg_d:
                            P.dma("sp", lambda e, bi=bi, dc=dc: e.dma_start(out=dbg_d["y0"][dc], in_=yout[bi][:]), reads=["yout%d" % bi], writes=["dbg"])
                    if stop_after == "F0":
                        break
                P.barrier()
                P.emit_block()
            if stop_after in ("F", "F0"):
                return nc

            with ExitStack() as st:
                g2b = sb(st, "g2b", [128, D])
                nfb = sb(st, "nfb", [128, D])
                x1g = sb(st, "x1g", [128, D])
                gt = [sb(st, "gt%d" % i, [128, D]) for i in range(8)]
                acc = sb(st, "acc", [128, D])
                tmpg = sb(st, "tmpg", [128, D])
                junk = sb(st, "junkG", [128, D], BF16)
                smg = sb(st, "smg", [128, 2])
                P.dma("sp", lambda e: e.dma_start(out=g2b[:], in_=bc_d[3:4, :].broadcast_to([128, D])), writes=["g2b"])
                P.dma("sp", lambda e: e.dma_start(out=nfb[:], in_=nfin_d.broadcast_to([128, D])), writes=["nfb"])
                P.op("pool", lambda e: e.memset(tmpg[:], 0.0), writes=["tmpg"])
                P.dma("sp", lambda e: e.dma_start(out=ys_d[NSLOT:NSLOT + 128, :], in_=tmpg[:]), reads=["tmpg"], writes=["ys_d"])
                for i in range(16):
                    tsl = slice(i * 128, (i + 1) * 128)
                    P.dma("sp", lambda e, tsl=tsl: e.dma_start(out=x1g[:], in_=x1_d[tsl, :]), reads=["x1_d"], writes=["x1g"])
                    if i == 0:
                        for k in range(4):
                            P.dma("pool", lambda e, k=k: e.indirect_dma_start(
                                out=gt[k][:], out_offset=None, in_=ys_d, in_offset=bass.IndirectOffsetOnAxis(ap=idx[:, 0, k:k + 1], axis=0)),
                                reads=["idx", "ys_d"], writes=["gt%d" % k])
                    if i + 1 < 16:
                        for k in range(4):
                            gn = ((i + 1) * 4 + k) % 8
                            P.dma("pool", lambda e, i=i, k=k, gn=gn: e.indirect_dma_start(
                                out=gt[gn][:], out_offset=None, in_=ys_d, in_offset=bass.IndirectOffsetOnAxis(ap=idx[:, i + 1, k:k + 1], axis=0)),
                                reads=["idx", "ys_d"], writes=["gt%d" % gn])
                    for k in range(4):
                        gi = (i * 4 + k) % 8
                        if k == 0:
                            P.op("dve", lambda e, i=i, k=k, gi=gi: e.tensor_scalar(acc[:], gt[gi][:], gk[:, i, k:k + 1], None, ALU.mult), reads=["gt%d" % gi, "gk"], writes=["acc"])
                        else:
                            P.op("dve", lambda e, i=i, k=k, gi=gi: e.tensor_scalar(tmpg[:], gt[gi][:], gk[:, i, k:k + 1], None, ALU.mult), reads=["gt%d" % gi, "gk"], writes=["tmpg"])
                            P.op("dve", lambda e: e.tensor_tensor(acc[:], acc[:], tmpg[:], ALU.add), reads=["acc", "tmpg"], writes=["acc"])
                    if i == 0 and "moe" in dbg_d:
                        P.dma("sp", lambda e: e.dma_start(out=dbg_d["moe"], in_=acc[:]), reads=["acc"], writes=["dbg"])
                    P.op("dve", lambda e: e.tensor_tensor(acc[:], acc[:], g2b[:], ALU.mult), reads=["acc", "g2b"], writes=["acc"])
                    P.op("dve", lambda e: e.tensor_tensor(acc[:], acc[:], x1g[:], ALU.add), reads=["acc", "x1g"], writes=["acc"])
                    P.op("act", lambda e: e.activation(junk[:], acc[:], AF.Square, accum_out=smg[:, 0:1]), reads=["acc"], writes=["junkG", "smg"])
                    P.op("act", lambda e: e.activation(smg[:, 0:1], smg[:, 0:1], AF.Ln, scale=1.0 / D, bias=EPS), reads=["smg"], writes=["smg"])
                    P.op("act", lambda e: e.activation(smg[:, 0:1], smg[:, 0:1], AF.Exp, scale=-0.5), reads=["smg"], writes=["smg"])
                    P.op("dve", lambda e: e.tensor_scalar(acc[:], acc[:], smg[:, 0:1], None, ALU.mult), reads=["acc", "smg"], writes=["acc"])
                    P.op("dve", lambda e: e.tensor_tensor(acc[:], acc[:], nfb[:], ALU.mult), reads=["acc", "nfb"], writes=["acc"])
                    P.dma("sp", lambda e, tsl=tsl: e.dma_start(out=out_d[tsl, :], in_=acc[:]), reads=["acc"], writes=["out_d"])
                P.barrier()
                P.emit_block()

    return nc


def _prep_inputs(inputs):
    x = np.asarray(inputs["x"], np.float32)
    consts = _host_consts()
    drow, dcol = _na_gather_idx()
    rpb = np.asarray(inputs["rpb"], np.float32)[0]
    nag = np.ascontiguousarray(rpb[:, drow, dcol].reshape(NH, 128, 7, 2, 64).transpose(0, 1, 3, 2, 4))
    shared = dict(
        w_ada=np.ascontiguousarray(inputs["w_ada"][0]),
        b_ada=np.ascontiguousarray(inputs["b_ada"][0][None, :]),
        norm_ffn=np.ascontiguousarray(inputs["norm_ffn"][0][None, :]),
        norm_final=np.ascontiguousarray(np.asarray(inputs["norm_final"])[None, :]),
        w_in=np.ascontiguousarray(inputs["w_in"][0]),
        hg_norm=np.ascontiguousarray(inputs["hg_norm"][0][:, None]),
        nag=nag,
        w_out=np.ascontiguousarray(inputs["w_out"][0]),
        w_router=np.ascontiguousarray(inputs["w_router"][0]),
        b_router=np.ascontiguousarray(inputs["b_router"][0][None, :]),
        w_gu=np.ascontiguousarray(inputs["w_gu"][0]),
        b_gu=np.ascontiguousarray(inputs["b_gu"][0]),
        w_down=np.ascontiguousarray(inputs["w_down"][0]),
        b_down=np.ascontiguousarray(inputs["b_down"][0]),
    )
    shared.update(consts)
    maps = []
    for b in range(x.shape[0]):
        rows8 = np.zeros((8, D), np.float32)
        rows8[0] = inputs["norm_mix"][0]
        rows8[1] = inputs["lb_table"][0]
        rows8[2] = inputs["lb_table"][1]
        rows8[3] = inputs["c"][b]
        rows8[4] = inputs["c_ctx"]
        m = dict(shared)
        m["x"] = np.ascontiguousarray(x[b])
        m["ctx"] = np.ascontiguousarray(inputs["ctx"][b])
        m["rows8"] = rows8
        maps.append(m)
    return maps


def kernel(**inputs):
    maps = _prep_inputs(inputs)
    nc = build()
    res = run_bass_kernel_spmd(nc, maps, core_ids=list(range(len(maps))))
    return np.stack([r["out"] for r in res.results], axis=0)
```

```python
import numpy as np
from contextlib import ExitStack
import concourse.bass as bass
import concourse.mybir as mybir
from concourse.bass_utils import run_bass_kernel_spmd

F32 = mybir.dt.float32
BF16 = mybir.dt.bfloat16
I32 = mybir.dt.int32
U32 = mybir.dt.uint32
AF = mybir.ActivationFunctionType
ALU = mybir.AluOpType
AX = mybir.AxisListType

ENG_NAMES = ("pe", "act", "dve", "pool", "sp")
N_DMA_SEMS = 6

D = 2048
T = 2048
LC = 256
NH = 8
DH = 128
NE = 32
CAP = 768
CH = 32
EPS = 1e-6
NEG = -30000.0
import os
CUT = int(os.environ.get('KCUT', '99'))


class _Cut(Exception):
    pass


_PROG = [None]


def cut(n):
    if CUT == n:
        _PROG[0].disabled = True


class Prog:
    def __init__(self, nc, st):
        self.nc = nc
        self.thunks = {e: [] for e in ENG_NAMES}
        self.count = {e: 0 for e in ENG_NAMES}
        self.waited = {e: {} for e in ENG_NAMES}
        self.bufs = {}
        self.dma_rr = {e: 0 for e in ENG_NAMES}
        self.dma_val = {}
        self.semkeys = list(ENG_NAMES)
        for q in ("sp", "act", "pool"):
            for j in range(N_DMA_SEMS):
                k = "d_%s_%d" % (q, j)
                self.semkeys.append(k)
                self.dma_val[k] = 0
        self.sems = {}
        for k in self.semkeys:
            self.sems[k] = st.enter_context(nc.semaphore("s_" + k))
        self.ninstr = 0
        self.disabled = False
        _PROG[0] = self

    def _deps(self, reads, writes):
        deps = {}

        def add(pid):
            if pid is None:
                return
            k, v = pid
            if deps.get(k, 0) < v:
                deps[k] = v
        for key in reads:
            b = self.bufs.get(key)
            if b is not None:
                add(b[0])
        for key in writes:
            b = self.bufs.get(key)
            if b is not None:
                add(b[0])
                for r in b[1]:
                    add(r)
        return deps

    def _update(self, pid, reads, writes):
        for key in reads:
            b = self.bufs.setdefault(key, [None, []])
            b[1].append(pid)
            if len(b[1]) > 48:
                m = {}
                for (k, v) in b[1]:
                    if m.get(k, 0) < v:
                        m[k] = v
                b[1] = list(m.items())
        for key in writes:
            self.bufs[key] = [pid, []]

    def _waits_for(self, eng, deps):
        out = []
        w = self.waited[eng]
        for k, v in deps.items():
            if w.get(k, 0) < v:
                w[k] = v
                out.append((k, v))
        return out

    def op(self, eng, fn, reads=(), writes=()):
        if self.disabled:
            return None
        deps = self._deps(reads, writes)
        waits = self._waits_for(eng, deps)
        self.count[eng] += 1
        pid = (eng, self.count[eng])
        self._update(pid, reads, writes)
        sems = self.sems

        def thunk(e):
            for k, v in waits:
                e.wait_ge(sems[k], v)
            ins = fn(e)
            ins.then_inc(sems[eng], 1)
        self.thunks[eng].append(thunk)
        self.ninstr += 1
        return pid

    def dma(self, q, fn, reads=(), writes=()):
        if self.disabled:
            return None
        j = self.dma_rr[q]
        self.dma_rr[q] = (j + 1) % N_DMA_SEMS
        k = "d_%s_%d" % (q, j)
        deps = self._deps(reads, writes)
        prev = self.dma_val[k]
        if prev > 0 and deps.get(k, 0) < prev:
            deps[k] = prev
        waits = self._waits_for(q, deps)
        self.dma_val[k] = prev + 16
        pid = (k, prev + 16)
        self._update(pid, reads, writes)
        sems = self.sems

        def thunk(e):
            for kk, v in waits:
                e.wait_ge(sems[kk], v)
            ins = fn(e)
            ins.then_inc(sems[k], 16)
        self.thunks[q].append(thunk)
        self.ninstr += 1
        return pid

    def barrier(self):
        deps = {}
        for e in ENG_NAMES:
            if self.count[e] > 0:
                deps[e] = self.count[e]
        for k, v in self.dma_val.items():
            if v > 0:
                deps[k] = v
        sems = self.sems
        for eng in ENG_NAMES:
            waits = self._waits_for(eng, dict(deps))

            def thunk(e, waits=waits):
                for k, v in waits:
                    e.wait_ge(sems[k], v)
            self.thunks[eng].append(thunk)
        self.bufs = {}

    def emit_block(self):
        nc = self.nc
        th = self.thunks
        with nc.Block() as block:
            @block.tensor
            def _(e):
                for t in th["pe"]:
                    t(e)

            @block.scalar
            def _(e):
                for t in th["act"]:
                    t(e)

            @block.vector
            def _(e):
                for t in th["dve"]:
                    t(e)

            @block.gpsimd
            def _(e):
                for t in th["pool"]:
                    t(e)

            @block.sync
            def _(e):
                for t in th["sp"]:
                    t(e)
        self.thunks = {e: [] for e in ENG_NAMES}


def _host_consts():
    c = {}
    c["ident"] = np.eye(128, dtype=np.float32)
    perm = np.zeros((128, 128), np.float32)
    for m in range(128):
        perm[m ^ 32, m] = 1.0
    c["perm"] = perm
    t = np.arange(T)
    nf = DH // 4
    inv = (10000.0 ** (-np.arange(nf, dtype=np.float32) / nf)).astype(np.float32)
    ang_row = (t // 64).astype(np.float32)[:, None] * inv[None, :]
    ang_col = (t % 64).astype(np.float32)[:, None] * inv[None, :]
    C = np.zeros((128, T), np.float32)
    S = np.zeros((128, T), np.float32)
    for blk, ang in ((0, ang_row), (64, ang_col)):
        cs = np.cos(ang).astype(np.float32).T
        sn = np.sin(ang).astype(np.float32).T
        C[blk:blk + 32] = cs
        C[blk + 32:blk + 64] = cs
        S[blk:blk + 32] = -sn
        S[blk + 32:blk + 64] = sn
    c["ropeC"] = C
    c["ropeS"] = S
    qc = np.arange(64)
    col_start = np.clip(qc - 8, 0, 48)
    kc = np.arange(64)
    inw = (kc[:, None] >= col_start[None, :]) & (kc[:, None] < col_start[None, :] + 16)
    m = np.where(inw, 0.0, NEG).astype(np.float32)
    c["namask"] = np.concatenate([m, m], axis=0)
    s_ = np.arange(CH)
    mf = (s_[:, None] <= s_[None, :]).astype(np.float32)
    mb = (s_[:, None] >= s_[None, :]).astype(np.float32)
    a_ = np.arange(128)
    same = (a_[:, None] // CH) == (a_[None, :] // CH)
    bf_ = (same & (a_[:, None] <= a_[None, :])).astype(np.float32)
    bb_ = (same & (a_[:, None] >= a_[None, :])).astype(np.float32)
    c["trif"] = np.ascontiguousarray(np.broadcast_to(bf_[:, None, :], (128, 4, 128)))
    c["trib"] = np.ascontiguousarray(np.broadcast_to(bb_[:, None, :], (128, 4, 128)))
    c["iotae"] = np.ascontiguousarray(np.broadcast_to(np.arange(NE, dtype=np.float32)[None, :], (128, NE)))
    k_ = np.arange(128)
    c["lstrict"] = (k_[:, None] < k_[None, :]).astype(np.float32)
    return c


def _na_gather_idx():
    j = np.arange(2)[:, None, None, None]
    kc = np.arange(64)[None, :, None, None]
    d = np.arange(14)[None, None, :, None]
    qc = np.arange(64)[None, None, None, :]
    drow = np.broadcast_to(d + j, (2, 64, 14, 64)).reshape(128, 14, 64)
    dcol = np.broadcast_to(np.clip(kc - qc + 15, 0, 30), (2, 64, 14, 64)).reshape(128, 14, 64)
    return drow, dcol


def build(dbg=None, stop_after=None):
    dbg = dbg or {}
    nc = bass.Bass("TRN2", target_bir_lowering=False)

    def din(name, shape, dt=F32):
        return nc.dram_tensor(name, list(shape), dt, kind="ExternalInput").ap()

    x_d = din("x", [T, D])
    ctx_d = din("ctx", [LC, D])
    rows_d = din("rows8", [8, D])
    w_ada_d = din("w_ada", [D, 6 * D])
    b_ada_d = din("b_ada", [1, 6 * D])
    nffn_d = din("norm_ffn", [1, D])
    nfin_d = din("norm_final", [1, D])
    w_in_d = din("w_in", [D, 8 * 1024])
    hgn_d = din("hg_norm", [128, 1])
    nag_d = din("nag", [NH, 128, 2, 7, 64])
    w_out_d = din("w_out", [D, D])
    w_r_d = din("w_router", [D, NE])
    b_r_d = din("b_router", [1, NE])
    need_moe = stop_after in (None, 'F', 'F0', 'G')
    if need_moe:
        w_gu_d = din("w_gu", [NE, D, 2 * D])
        b_gu_d = din("b_gu", [NE, 2 * D])
        w_dn_d = din("w_down", [NE, D, D])
        b_dn_d = din("b_down", [NE, D])
    ident_d = din("ident", [128, 128])
    perm_d = din("perm", [128, 128])
    ropeC_d = din("ropeC", [128, T])
    ropeS_d = din("ropeS", [128, T])
    namask_d = din("namask", [128, 64])
    trif_d = din("trif", [128, 4, 128])
    trib_d = din("trib", [128, 4, 128])
    iotae_d = din("iotae", [128, NE])
    lstrict_d = din("lstrict", [128, 128])
    out_d = nc.dram_tensor("out", [T, D], F32, kind="ExternalOutput").ap()
    dbg_d = {k: nc.dram_tensor("dbg_" + k, list(shp[0]), shp[1], kind="ExternalOutput").ap() for k, shp in dbg.items()}

    bc_d = nc.dram_tensor("bc_scr", [5, D], F32, kind="Internal").ap()
    cat_d = nc.dram_tensor("cat_scr", [16, 128, T], BF16, kind="Internal").ap()
    x1_d = nc.dram_tensor("x1_scr", [T, D], F32, kind="Internal").ap()
    xs_d = nc.dram_tensor("xs_scr", [NE * CAP + 128, D], BF16, kind="Internal").ap()
    ys_d = nc.dram_tensor("ys_scr", [NE * CAP + 128, D], F32, kind="Internal").ap()

    with ExitStack() as top:
        P = Prog(nc, top)

        def sb(st, name, shape, dt=F32):
            return st.enter_context(nc.sbuf_tensor("sb_" + name, list(shape), dt))

        def ps(st, name, shape, dt=F32):
            return st.enter_context(nc.psum_tensor("ps_" + name, list(shape), dt))

        identf = sb(top, "identf", [128, 128])
        identb = sb(top, "identb", [128, 128], BF16)
        onesb = sb(top, "onesb", [128, 128], BF16)
        onesf = sb(top, "onesf", [128, 128])
        vecF = sb(top, "vecF", [128, 16, 8])
        modF = sb(top, "modF", [128, 96, 2])
        A1 = sb(top, "A1", [128, 16])
        Ac = sb(top, "Ac", [128, 16])
        lbF = sb(top, "lbF", [128, 16])
        omlF = sb(top, "omlF", [128, 16])
        nomlF = sb(top, "nomlF", [128, 16])
        hgF = sb(top, "hgF", [128, 1])

        P.dma("sp", lambda e: e.dma_start(out=identf[:], in_=ident_d), writes=["identf"])
        P.dma("pool", lambda e: e.dma_start(out=identb[:], in_=ident_d), writes=["identb"])
        P.op("dve", lambda e: e.memset(onesb[:], 1.0), writes=["onesb"])
        P.op("dve", lambda e: e.memset(onesf[:], 1.0), writes=["onesf"])
        P.dma("sp", lambda e: e.dma_start(out=hgF[:], in_=hgn_d), writes=["hgF"])

        def debug_out(name, ap, key):
            if name in dbg_d:
                P.dma("sp", lambda e: e.dma_start(out=dbg_d[name], in_=ap), reads=[key], writes=["dbg_" + name])

        with ExitStack() as st:
            rows8 = sb(st, "rows8", [8, D])
            sT = sb(st, "sT", [128, 16, 2])
            tmpA = sb(st, "tmpA", [128, 16, 2])
            mod_row = sb(st, "mod_row", [2, 6 * D])
            wa = [sb(st, "wa%d" % i, [128, 16, 512]) for i in range(2)]
            nrow = sb(st, "nrow", [1, D])
            a2row = sb(st, "a2row", [1, D])
            ps_v = ps(st, "ps_v", [128, 16, 8])
            ps_m = [ps(st, "ps_m%d" % i, [2, 512]) for i in range(2)]
            ps_f = ps(st, "ps_f", [128, 96, 2])

            P.dma("sp", lambda e: e.dma_start(out=rows8[:], in_=rows_d), writes=["rows8"])
            cut(0)
            P.dma("sp", lambda e: e.dma_start(out=mod_row[0:1, :], in_=b_ada_d), writes=["mod_row"])
            P.dma("sp", lambda e: e.dma_start(out=mod_row[1:2, :], in_=b_ada_d), writes=["mod_row"])
            P.dma("sp", lambda e: e.dma_start(out=nrow[:], in_=nffn_d), writes=["nrow"])

            def tr_rows(e):
                ins = None
                for k in range(16):
                    ins = e.transpose(ps_v[:, k, :], rows8[0:8, k * 128:(k + 1) * 128], identf[0:8, 0:8])
                return ins
            P.op("pe", tr_rows, reads=["rows8", "identf"], writes=["ps_v"])
            P.op("dve", lambda e: e.tensor_copy(vecF[:], ps_v[:]), reads=["ps_v"], writes=["vecF"])
            cut(1)
            P.op("act", lambda e: e.activation(tmpA[:], vecF[:, :, 3:5], AF.Exp, scale=-1.0), reads=["vecF"], writes=["tmpA"])
            P.op("dve", lambda e: e.tensor_scalar(tmpA[:], tmpA[:], 1.0, None, ALU.add), reads=["tmpA"], writes=["tmpA"])
            P.op("dve", lambda e: e.reciprocal(tmpA[:], tmpA[:]), reads=["tmpA"], writes=["tmpA"])
            P.op("dve", lambda e: e.tensor_tensor(sT[:], tmpA[:], vecF[:, :, 3:5], ALU.mult), reads=["tmpA", "vecF"], writes=["sT"])
            P.op("dve", lambda e: e.tensor_tensor(lbF[:], vecF[:, :, 2], vecF[:, :, 1], ALU.subtract), reads=["vecF"], writes=["lbF"])
            P.op("act", lambda e: e.activation(lbF[:], lbF[:], AF.Exp), reads=["lbF"], writes=["lbF"])
            P.op("dve", lambda e: e.tensor_scalar(lbF[:], lbF[:], 1.0, None, ALU.add), reads=["lbF"], writes=["lbF"])
            P.op("dve", lambda e: e.reciprocal(lbF[:], lbF[:]), reads=["lbF"], writes=["lbF"])
            P.op("dve", lambda e: e.tensor_scalar(omlF[:], lbF[:], -1.0, 1.0, ALU.mult, ALU.add), reads=["lbF"], writes=["omlF"])
            P.op("dve", lambda e: e.tensor_scalar(nomlF[:], lbF[:], -1.0, None, ALU.add), reads=["lbF"], writes=["nomlF"])

            cut(2)
            wav = w_ada_d.rearrange("(k p) n -> p k n", p=128)
            for n in range(24):
                bi = n % 2
                P.dma("sp", lambda e, n=n, bi=bi: e.dma_start(out=wa[bi][:], in_=wav[:, :, n * 512:(n + 1) * 512]), writes=["wa%d" % bi])

                def mmA(e, bi=bi):
                    ins = None
                    for k in range(16):
                        ins = e.matmul(ps_m[bi][:], sT[:, k, :], wa[bi][:, k, :], start=(k == 0), stop=(k == 15))
                    return ins
                P.op("pe", mmA, reads=["sT", "wa%d" % bi], writes=["ps_m%d" % bi])
                P.op("dve", lambda e, n=n, bi=bi: e.tensor_tensor(mod_row[:, n * 512:(n + 1) * 512], ps_m[bi][:], mod_row[:, n * 512:(n + 1) * 512], ALU.add),
                     reads=["ps_m%d" % bi, "mod_row"], writes=["mod_row"])

            cut(3)

            def tr_mod(e):
                ins = None
                for c in range(96):
                    ins = e.transpose(ps_f[:, c, :], mod_row[0:2, c * 128:(c + 1) * 128], identf[0:2, 0:2])
                return ins
            P.op("pe", tr_mod, reads=["mod_row", "identf"], writes=["ps_f"])
            P.op("dve", lambda e: e.tensor_copy(modF[:], ps_f[:]), reads=["ps_f"], writes=["modF"])
            P.op("dve", lambda e: e.scalar_tensor_tensor(A1[:], modF[:, 16:32, 0], 1.0, vecF[:, :, 0], ALU.add, ALU.mult), reads=["modF", "vecF"], writes=["A1"])
            P.op("dve", lambda e: e.scalar_tensor_tensor(Ac[:], modF[:, 16:32, 1], 1.0, vecF[:, :, 0], ALU.add, ALU.mult), reads=["modF", "vecF"], writes=["Ac"])
            cut(4)
            P.op("dve", lambda e: e.scalar_tensor_tensor(a2row[:], mod_row[0:1, 4 * D:5 * D], 1.0, nrow[:], ALU.add, ALU.mult), reads=["mod_row", "nrow"], writes=["a2row"])
            P.dma("sp", lambda e: e.dma_start(out=bc_d[0:1, :], in_=mod_row[0:1, 2 * D:3 * D]), reads=["mod_row"], writes=["bc_d"])
            P.dma("sp", lambda e: e.dma_start(out=bc_d[1:2, :], in_=a2row[:]), reads=["a2row"], writes=["bc_d"])
            P.dma("sp", lambda e: e.dma_start(out=bc_d[2:3, :], in_=mod_row[0:1, 3 * D:4 * D]), reads=["mod_row"], writes=["bc_d"])
            P.dma("sp", lambda e: e.dma_start(out=bc_d[3:4, :], in_=mod_row[0:1, 5 * D:6 * D]), reads=["mod_row"], writes=["bc_d"])
            debug_out("mod", mod_row[:], "mod_row")
            P.disabled = False
            P.barrier()
            P.emit_block()
        if stop_after == "A":
            return nc

        with ExitStack() as stBC:
            hxT = sb(stBC, "hxT", [128, 16, T], BF16)
            hcT = sb(stBC, "hcT", [128, 16, LC], BF16)
            with ExitStack() as st:
                xt = [sb(st, "xt%d" % i, [128, D]) for i in range(2)]
                xn = [sb(st, "xn%d" % i, [128, D]) for i in range(2)]
                junk = sb(st, "junk", [128, D], BF16)
                ss = [sb(st, "ss%d" % i, [128, 1]) for i in range(2)]
                tp = [ps(st, "tp%d" % i, [128, 4, 128]) for i in range(4)]
                tpi = 0
                for i in range(18):
                    bi = i % 2
                    isx = i < 16
                    src = x_d[i * 128:(i + 1) * 128, :] if isx else ctx_d[(i - 16) * 128:(i - 15) * 128, :]
                    dstT = hxT if isx else hcT
                    c0 = (i if isx else i - 16) * 128
                    Asc, bcol = (A1, 0) if isx else (Ac, 1)
                    P.dma("sp", lambda e, bi=bi, src=src: e.dma_start(out=xt[bi][:], in_=src), writes=["xt%d" % bi])
                    P.op("act", lambda e, bi=bi: e.activation(junk[:], xt[bi][:], AF.Square, accum_out=ss[bi][:]), reads=["xt%d" % bi], writes=["junk", "ss%d" % bi])
                    cut(10)
                    P.op("act", lambda e, bi=bi: e.activation(ss[bi][:], ss[bi][:], AF.Ln, scale=1.0 / D, bias=EPS), reads=["ss%d" % bi], writes=["ss%d" % bi])
                    P.op("act", lambda e, bi=bi: e.activation(ss[bi][:], ss[bi][:], AF.Exp, scale=-0.5), reads=["ss%d" % bi], writes=["ss%d" % bi])
                    cut(11)
                    P.op("dve", lambda e, bi=bi: e.tensor_scalar(xn[bi][:], xt[bi][:], ss[bi][:, 0:1], None, ALU.mult), reads=["xt%d" % bi, "ss%d" % bi], writes=["xn%d" % bi])
                    cut(12)
                    for kg in range(4):
                        tb = tpi % 4
                        tpi += 1

                        def trB(e, bi=bi, kg=kg, tb=tb):
                            ins = None
                            for kk in range(4):
                                k = kg * 4 + kk
                                ins = e.transpose(tp[tb][:, kk, :], xn[bi][:, k * 128:(k + 1) * 128], identf[:])
                            return ins
                        P.op("pe", trB, reads=["xn%d" % bi, "identf"], writes=["tp%d" % tb])
                        cut(13)
                        for kk in range(4):
                            k = kg * 4 + kk
                            if True:
                                P.op("act", lambda e, tb=tb, kk=kk, k=k, dstT=dstT, c0=c0, Asc=Asc, bcol=bcol: e.activation(
                                    dstT[:, k, c0:c0 + 128], tp[tb][:, kk, :], AF.Identity, scale=Asc[:, k:k + 1], bias=modF[:, k, bcol:bcol + 1]),
                                    reads=["tp%d" % tb, "A1", "Ac", "modF"], writes=["hT%d_%d" % (i, k)])
                                cut(14)
                            else:
                                P.op("dve", lambda e, tb=tb, kk=kk, k=k, dstT=dstT, c0=c0, Asc=Asc, bcol=bcol: e.tensor_scalar(
                                    dstT[:, k, c0:c0 + 128], tp[tb][:, kk, :], Asc[:, k:k + 1], modF[:, k, bcol:bcol + 1], ALU.mult, ALU.add),
                                    reads=["tp%d" % tb, "A1", "Ac", "modF"], writes=["hT%d_%d" % (i, k)])
                    cut(20 + i)
                P.disabled = False
                if "hxT" in dbg_d:
                    P.barrier()
                    P.dma("sp", lambda e: e.dma_start(out=dbg_d["hxT"], in_=hxT[:]), writes=["dbg"])
                P.barrier()
                P.emit_block()
            if stop_after == "B":
                return nc

            w_in_v = w_in_d.rearrange("(k p) n -> p k n", p=128)

            with ExitStack() as st:
                wts = [sb(st, "wts%d" % i, [128, 16, 128], BF16) for i in range(4)]
                wrr = [0]
                QT = sb(st, "QT", [128, T], BF16)
                KT = sb(st, "KT", [128, T + LC], BF16)
                Ve = sb(st, "Ve", [128, 18, 128], BF16)
                Vo = sb(st, "Vo", [128, 15, 128], BF16)
                nab = sb(st, "nab", [128, 2, 7, 64])
                namask = sb(st, "namask", [128, 64])
                sw = [sb(st, "sw%d" % i, [128, 4, 64]) for i in range(2)]
                pT = [sb(st, "pT%d" % i, [128, 6, 64], BF16) for i in range(2)]
                rd = [sb(st, "rd%d" % i, [128, 64]) for i in range(2)]
                catT = [sb(st, "catT%d" % i, [128, T], BF16) for i in range(2)]
                pj = [ps(st, "pj%d" % i, [128, 512]) for i in range(2)]
                pv = [ps(st, "pv%d" % i, [128, 4, 128]) for i in range(2)]
                scp = [ps(st, "scp%d" % i, [128, 6, 64]) for i in range(2)]
                ndp = [ps(st, "ndp%d" % i, [128, 2, 64]) for i in range(2)]
                pjr = [0]
                P.dma("sp", lambda e: e.dma_start(out=namask[:], in_=namask_d), writes=["namask"])

                def load_w(col0):
                    wi = wrr[0] % 4
                    wrr[0] += 1
                    P.dma("pool", lambda e: e.dma_start(out=wts[wi][:], in_=w_in_v[:, :, col0:col0 + 128]), writes=["wts%d" % wi])
                    return wi

                def proj_fm(wi, srcT, srckey, n0, n, evac):
                    pi = pjr[0] % 2
                    pjr[0] += 1

                    def f(e):
                        ins = None
                        for k in range(16):
                            ins = e.matmul(pj[pi][:, 0:n], wts[wi][:, k, :], srcT[:, k, n0:n0 + n], start=(k == 0), stop=(k == 15))
                        return ins
                    P.op("pe", f, reads=["wts%d" % wi, srckey], writes=["pj%d" % pi])
                    evac(pj[pi][:, 0:n], "pj%d" % pi)

                pvr = [0]

                def proj_tm(wi, tiles, dst, dstkey):
                    for g0 in range(0, len(tiles), 4):
                        grp = tiles[g0:g0 + 4]
                        pi = pvr[0] % 2
                        pvr[0] += 1

                        def f(e, grp=grp, pi=pi):
                            ins = None
                            for gi, (srcT, srckey, tok0) in enumerate(grp):
                                for k in range(16):
                                    ins = e.matmul(pv[pi][:, gi, :], srcT[:, k, tok0:tok0 + 128], wts[wi][:, k, :], start=(k == 0), stop=(k == 15))
                            return ins
                        P.op("pe", f, reads=["wts%d" % wi] + [t[1] for t in grp], writes=["pv%d" % pi])
                        ng = len(grp)
                        P.op("dve", lambda e, pi=pi, g0=g0, ng=ng: e.tensor_copy(dst[:, g0:g0 + ng, :], pv[pi][:, 0:ng, :]), reads=["pv%d" % pi], writes=[dstkey])

                xkeys = "hxT"
                for h in range(NH if stop_after != "C2a" else 0):
                    ci = h % 2
                    wq = load_w(0 * 1024 + h * 128)
                    wk = load_w(1 * 1024 + h * 128)
                    wv = load_w(2 * 1024 + h * 128)
                    P.dma("sp", lambda e, h=h: e.dma_start(out=nab[:], in_=nag_d[h]), writes=["nab"])
                    for par in range(2):
                        for m in range(7):
                            P.op("pool", lambda e, par=par, m=m: e.tensor_tensor(nab[:, par, m, :], nab[:, par, m, :], namask[:], ALU.add), reads=["nab", "namask"], writes=["nab"])
                    for c4 in range(4):
                        proj_fm(wq, hxT, "hxT", c4 * 512, 512, lambda pap, pk, c4=c4: P.op(
                            "act", lambda e: e.activation(QT[:, c4 * 512:(c4 + 1) * 512], pap, AF.Copy, scale=DH ** -0.5), reads=[pk], writes=["QT"]))
                        proj_fm(wk, hxT, "hxT", c4 * 512, 512, lambda pap, pk, c4=c4: P.op(
                            "dve", lambda e: e.tensor_copy(KT[:, c4 * 512:(c4 + 1) * 512], pap), reads=[pk], writes=["KT"]))
                    proj_fm(wk, hcT, "hcT", 0, LC, lambda pap, pk: P.op(
                        "dve", lambda e: e.tensor_copy(KT[:, T:T + LC], pap), reads=[pk], writes=["KT"]))
                    proj_tm(wv, [(hxT, "hxT", i * 128) for i in range(16)] + [(hcT, "hcT", 0), (hcT, "hcT", 128)], Ve, "Ve")
                    proj_tm(wv, [(hxT, "hxT", 64 + i * 128) for i in range(15)], Vo, "Vo")
                    for r in range(32):
                        bi = r % 2
                        ks = min(max(r - 4, 0), 24)
                        d0 = ks - r + 7
                        par, m0 = d0 % 2, d0 // 2

                        def fqk(e, r=r, ks=ks, bi=bi):
                            ins = None
                            for j in range(6):
                                k0 = (ks + 2 * j) * 64 if j < 4 else T + (j - 4) * 128
                                ins = e.matmul(scp[bi][:, j, :], KT[:, k0:k0 + 128], QT[:, r * 64:(r + 1) * 64], start=True, stop=True)
                            return ins
                        P.op("pe", fqk, reads=["QT", "KT"], writes=["scp%d" % bi])
                        P.op("dve", lambda e, bi=bi, par=par, m0=m0: e.tensor_tensor(sw[bi][:], scp[bi][:, 0:4, :], nab[:, par, m0:m0 + 4, :], ALU.add),
                             reads=["scp%d" % bi, "nab"], writes=["sw%d" % bi])
                        P.op("act", lambda e, bi=bi: e.activation(pT[bi][:, 0:4, :], sw[bi][:], AF.Exp), reads=["sw%d" % bi], writes=["pTa%d" % bi])
                        P.op("act", lambda e, bi=bi: e.activation(pT[bi][:, 4:6, :], scp[bi][:, 4:6, :], AF.Exp), reads=["scp%d" % bi], writes=["pTb%d" % bi])

                        def fpv(e, r=r, ks=ks, bi=bi):
                            ins = None
                            for j in range(6):
                                if j < 4:
                                    kr_ = ks + 2 * j
                                    vt = Ve[:, kr_ // 2, :] if kr_ % 2 == 0 else Vo[:, (kr_ - 1) // 2, :]
                                else:
                                    vt = Ve[:, 16 + (j - 4), :]
                                ins = e.matmul(ndp[bi][:, 0, :], vt, pT[bi][:, j, :], start=(j == 0), stop=(j == 5))
                            for j in range(6):
                                ins = e.matmul(ndp[bi][:, 1, :], onesb[:], pT[bi][:, j, :], start=(j == 0), stop=(j == 5))
                            return ins
                        P.op("pe", fpv, reads=["Ve", "Vo", "pTa%d" % bi, "pTb%d" % bi, "onesb"], writes=["ndp%d" % bi])
                        P.op("dve", lambda e, bi=bi: e.reciprocal(rd[bi][:], ndp[bi][:, 1, :]), reads=["ndp%d" % bi], writes=["rd%d" % bi])
                        P.op("dve", lambda e, bi=bi, r=r, ci=ci: e.tensor_tensor(catT[ci][:, r * 64:(r + 1) * 64], ndp[bi][:, 0, :], rd[bi][:], ALU.mult),
                             reads=["ndp%d" % bi, "rd%d" % bi], writes=["catT%d" % ci])
                    P.dma("sp", lambda e, h=h, ci=ci: e.dma_start(out=cat_d[h], in_=catT[ci][:]), reads=["catT%d" % ci], writes=["cat_d%d" % h])
                    if h == 0 and "na0" in dbg_d:
                        P.dma("sp", lambda e, ci=ci: e.dma_start(out=dbg_d["na0"], in_=catT[ci][:]), reads=["catT%d" % ci], writes=["dbg"])
                    if stop_after == "C1a" and h == 0:
                        break
                P.barrier()
                P.emit_block()
            if stop_after in ("C1", "C1a"):
                return nc

            TE = T + LC
            NCK = TE // CH
            with ExitStack() as st:
                wts = [sb(st, "wth%d" % i, [128, 16, 128], BF16) for i in range(3)]
                wrr = [0]
                B0 = sb(st, "B0", [128, TE])
                B1 = sb(st, "B1", [128, TE])
                B2 = sb(st, "B2", [128, TE])
                B3 = sb(st, "B3", [128, TE])
                B5 = sb(st, "B5", [128, T])
                oacc = sb(st, "oacc", [128, T])
                tmpq = [sb(st, "tmpq%d" % i, [128, 512]) for i in range(2)]
                rC = sb(st, "rC", [128, T], BF16)
                rS = sb(st, "rS", [128, T], BF16)
                smask = sb(st, "smask", [128, TE], BF16)
                permf = sb(st, "permf", [128, 128])
                bdm = [sb(st, "bdm%d" % i, [128, 4, 128]) for i in range(2)]
                kp = sb(st, "kp", [128, TE], BF16)
                kdfm = sb(st, "kdfm", [128, TE], BF16)
                qp = sb(st, "qp", [128, T], BF16)
                Vh = sb(st, "Vh", [128, 18, 128], BF16)
                kdT = sb(st, "kdT", [128, 18, 128], BF16)
                AT = sb(st, "AT", [128, 16, 128], BF16)
                Sb = sb(st, "Sb", [128, 64, 128], BF16)
                S32 = sb(st, "S32", [128, 4, 128])
                tot = sb(st, "tot", [128, NCK])
                pj = [ps(st, "hpj%d" % i, [128, 512]) for i in range(2)]
                pv = [ps(st, "hpv%d" % i, [128, 4, 128]) for i in range(2)]
                pu = [ps(st, "hpu%d" % i, [128, 4, 128]) for i in range(4)]
                pjr = [0]
                pvr = [0]
                P.dma("pool", lambda e: e.dma_start(out=rC[:], in_=ropeC_d), writes=["rC"])
                P.dma("pool", lambda e: e.dma_start(out=rS[:], in_=ropeS_d), writes=["rS"])
                P.dma("sp", lambda e: e.dma_start(out=permf[:], in_=perm_d), writes=["permf"])
                P.dma("sp", lambda e: e.dma_start(out=bdm[0][:], in_=trif_d), writes=["bdm0"])
                P.dma("sp", lambda e: e.dma_start(out=bdm[1][:], in_=trib_d), writes=["bdm1"])
                smv = smask[:].rearrange("p (c s) -> p c s", s=CH)
                P.op("pool", lambda e: e.memset(smask[:], 1.0), writes=["smask"])
                P.op("pool", lambda e: e.memset(smv[:, :, 0:1], 0.0), writes=["smask"])
                cut(40)

                def v3(buf):
                    return buf[:].rearrange("p (c s) -> p c s", s=CH)

                def load_wh(col0):
                    wi = wrr[0] % 3
                    wrr[0] += 1
                    P.dma("pool", lambda e: e.dma_start(out=wts[wi][:], in_=w_in_v[:, :, col0:col0 + 128]), writes=["wth%d" % wi])
                    return wi

                def proj_fm(wi, srcT, srckey, n0, n, evac):
                    pi = pjr[0] % 2
                    pjr[0] += 1

                    def f(e):
                        ins = None
                        for k in range(16):
                            ins = e.matmul(pj[pi][:, 0:n], wts[wi][:, k, :], srcT[:, k, n0:n0 + n], start=(k == 0), stop=(k == 15))
                        return ins
                    P.op("pe", f, reads=["wth%d" % wi, srckey], writes=["hpj%d" % pi])
                    evac(pj[pi][:, 0:n], "hpj%d" % pi)

                def mm_f32(lhsT, lkey, rhs, rkey, evac):
                    pi = pjr[0] % 2
                    pjr[0] += 1
                    n = rhs.shape[1]
                    P.op("pe", lambda e: e.matmul(pj[pi][:, 0:n], lhsT, rhs, start=True, stop=True), reads=[lkey, rkey], writes=["hpj%d" % pi])
                    evac(pj[pi][:, 0:n], "hpj%d" % pi)

                for h in range(NH):
                    wqh = load_wh(3 * 1024 + h * 128)
                    wih = load_wh(6 * 1024 + h * 128)
                    tiles = [(hxT, "hxT", i * 128) for i in range(16)] + [(hcT, "hcT", 0), (hcT, "hcT", 128)]
                    for g0 in range(0, 18, 4):
                        grp = tiles[g0:g0 + 4]
                        pi = pvr[0] % 2
                        pvr[0] += 1

                        def f(e, grp=grp, pi=pi, wih=wih):
                            ins = None
                            for gi, (srcT, srckey, tok0) in enumerate(grp):
                                for k in range(16):
                                    ins = e.matmul(pv[pi][:, gi, :], srcT[:, k, tok0:tok0 + 128], wts[wih][:, k, :], start=(k == 0), stop=(k == 15))
                            return ins
                        P.op("pe", f, reads=["wth%d" % wih, "hxT", "hcT"], writes=["hpv%d" % pi])
                        ng = len(grp)
                        P.op("act", lambda e, pi=pi, g0=g0, ng=ng: e.activation(Vh[:, g0:g0 + ng, :], pv[pi][:, 0:ng, :], AF.Copy), reads=["hpv%d" % pi], writes=["Vh"])
                    cut(41)
                    for c4 in range(4):
                        rng = slice(c4 * 512, (c4 + 1) * 512)
                        proj_fm(wqh, hxT, "hxT", c4 * 512, 512, lambda pap, pk, rng=rng, c4=c4: P.op(
                            "dve", lambda e: e.tensor_copy(B5[:, rng], pap), reads=[pk], writes=["B5_%d" % c4]))
                    for c4 in range(4):
                        rng = slice(c4 * 512, (c4 + 1) * 512)
                        tq = tmpq[c4 % 2]
                        tk = "tmpq%d" % (c4 % 2)

                        def evq(pap, pk, rng=rng, c4=c4, tq=tq, tk=tk):
                            P.op("dve", lambda e: e.tensor_tensor(tq[:], pap, rS[:, rng], ALU.mult), reads=[pk, "rS"], writes=[tk])
                            P.op("pool", lambda e: e.tensor_tensor(B5[:, rng], B5[:, rng], rC[:, rng], ALU.mult), reads=["B5_%d" % c4, "rC"], writes=["B5_%d" % c4])
                            P.op("pool", lambda e: e.tensor_tensor(B5[:, rng], B5[:, rng], tq[:], ALU.add), reads=["B5_%d" % c4, tk], writes=["B5_%d" % c4])
                        mm_f32(permf[:], "permf", B5[:, rng], "B5_%d" % c4, evq)
                    B5k = ["B5_%d" % c for c in range(4)]
                    cut(42)

                    for dd in range(2):
                        wf = load_wh((4 + dd) * 1024 + h * 128)
                        lc = dd * 8 + h
                        lb_ap, oml_ap, noml_ap = lbF[:, lc:lc + 1], omlF[:, lc:lc + 1], nomlF[:, lc:lc + 1]
                        for c4 in range(4):
                            rng = slice(c4 * 512, (c4 + 1) * 512)
                            proj_fm(wf, hxT, "hxT", c4 * 512, 512, lambda pap, pk, rng=rng: P.op(
                                "act", lambda e: e.activation(B0[:, rng], pap, AF.Exp, scale=-1.0), reads=[pk], writes=["B0"]))
                        proj_fm(wf, hcT, "hcT", 0, LC, lambda pap, pk: P.op(
                            "act", lambda e: e.activation(B0[:, T:TE], pap, AF.Exp, scale=-1.0), reads=[pk], writes=["B0"]))
                        P.op("dve", lambda e: e.tensor_scalar(B0[:], B0[:], 1.0, None, ALU.add), reads=["B0"], writes=["B0"])
                        P.op("dve", lambda e: e.reciprocal(B0[:], B0[:]), reads=["B0"], writes=["B0"])
                        P.op("act", lambda e, oml_ap=oml_ap, lb_ap=lb_ap: e.activation(B1[:], B0[:], AF.Ln, scale=oml_ap, bias=lb_ap), reads=["B0", "lbF", "omlF"], writes=["B1"])
                        P.op("pool", lambda e, oml_ap=oml_ap, noml_ap=noml_ap: e.tensor_scalar(B0[:], B0[:], noml_ap, oml_ap, ALU.mult, ALU.add), reads=["B0", "omlF", "nomlF"], writes=["B0"])
                        cut(43)
                        P.op("dve", lambda e: e.tensor_tensor_scan(B2[:], smask[:], B1[:], 0.0, ALU.mult, ALU.add), reads=["smask", "B1"], writes=["B2"])
                        cut(44)
                        if dd == 1:
                            P.op("dve", lambda e: e.tensor_tensor(B2[:], B2[:], B1[:], ALU.subtract), reads=["B2", "B1"], writes=["B2"])
                            P.op("dve", lambda e: e.tensor_tensor(tot[:], v3(B2)[:, :, CH - 1], v3(B1)[:, :, CH - 1], ALU.add), reads=["B2", "B1"], writes=["tot"])
                            P.op("dve", lambda e: e.tensor_tensor(v3(B2), tot[:].unsqueeze(2).to_broadcast([128, NCK, CH]), v3(B2), ALU.subtract), reads=["B2", "tot"], writes=["B2"])
                        P.op("act", lambda e: e.activation(B1[:], B2[:], AF.Exp), reads=["B2"], writes=["B1"])
                        P.op("act", lambda e: e.activation(B2[:], B2[:], AF.Exp, scale=-1.0), reads=["B2"], writes=["B2"])
                        cut(45)
                        for c4 in range(4):
                            rng = slice(c4 * 512, (c4 + 1) * 512)
                            tq = tmpq[c4 % 2]
                            tk = "tmpq%d" % (c4 % 2)

                            def evk(pap, pk, rng=rng, tq=tq, tk=tk):
                                P.op("dve", lambda e: e.tensor_tensor(tq[:], pap, rS[:, rng], ALU.mult), reads=[pk, "rS"], writes=[tk])
                                P.op("pool", lambda e: e.tensor_tensor(B3[:, rng], B0[:, rng], rC[:, rng], ALU.mult), reads=["B0", "rC"], writes=["B3"])
                                P.op("pool", lambda e: e.tensor_tensor(B3[:, rng], B3[:, rng], tq[:], ALU.add), reads=["B3", tk], writes=["B3"])
                            mm_f32(permf[:], "permf", B0[:, rng], "B0", evk)
                        P.op("pool", lambda e: e.tensor_copy(B3[:, T:TE], B0[:, T:TE]), reads=["B0"], writes=["B3"])
                        cut(46)
                        P.op("pool", lambda e: e.tensor_tensor(B3[:], B3[:], B2[:], ALU.mult), reads=["B3", "B2"], writes=["B3"])
                        P.op("act", lambda e: e.activation(kp[:], B3[:], AF.Copy), reads=["B3"], writes=["kp"])
                        di = CH - 1 if dd == 0 else 0
                        P.op("dve", lambda e, di=di: e.tensor_tensor(v3(kdfm), v3(B3), v3(B1)[:, :, di:di + 1].to_broadcast([128, NCK, CH]), ALU.mult), reads=["B3", "B1"], writes=["kdfm"])
                        P.op("dve", lambda e: e.tensor_tensor(qp[:], B5[:], B1[:, 0:T], ALU.mult), reads=B5k + ["B1"], writes=["qp"])
                        cut(47)
                        for g0 in range(0, 18, 4):
                            ng = min(4, 18 - g0)

                            pi = pvr[0] % 2
                            pvr[0] += 1
                            ptr = pv[pi][:].rearrange("p a b -> p (a b)").bitcast(BF16)[:, 0:512].rearrange("p (a b) -> p a b", b=128)

                            def ftr(e, g0=g0, ng=ng, ptr=ptr):
                                ins = None
                                for gi in range(ng):
                                    g = g0 + gi
                                    ins = e.transpose(ptr[:, gi, :], kdfm[:, g * 128:(g + 1) * 128], identb[:])
                                return ins
                            P.op("pe", ftr, reads=["kdfm", "identb"], writes=["hpv%d" % pi])
                            P.op("act", lambda e, g0=g0, ng=ng, ptr=ptr: e.activation(kdT[:, g0:g0 + ng, :], ptr[:, 0:ng, :], AF.Copy), reads=["hpv%d" % pi], writes=["kdT"])
                        cut(48)
                        for g0 in range(0, 16, 4):
                            pi = pvr[0] % 2
                            pvr[0] += 1

                            def fa(e, g0=g0, pi=pi):
                                ins = None
                                for gi in range(4):
                                    g = g0 + gi
                                    ins = e.matmul(pv[pi][:, gi, :], kp[:, g * 128:(g + 1) * 128], qp[:, g * 128:(g + 1) * 128], start=True, stop=True)
                                return ins
                            P.op("pe", fa, reads=["kp", "qp"], writes=["hpv%d" % pi])
                            P.op("dve", lambda e, g0=g0, pi=pi, dd=dd: e.tensor_tensor(AT[:, g0:g0 + 4, :], pv[pi][:], bdm[dd][:], ALU.mult), reads=["hpv%d" % pi, "bdm%d" % dd], writes=["AT"])
                        cut(49)
                        order = (list(range(64, 72)) + list(range(0, 64))) if dd == 0 else (list(range(71, 63, -1)) + list(range(63, -1, -1)))
                        P.op("pool", lambda e: e.memset(S32[:, 0, :], 0.0), writes=["S32_0"])
                        first_x = order[8]
                        for n0 in range(0, NCK, 4):
                            cs = order[n0:n0 + 4]
                            for j, c in enumerate(cs):
                                n = n0 + j
                                ui = c % 4
                                sl = (c // 4) % 4
                                pb = (c % 4) * 32
                                if os.environ.get("KSKIP") != "umm":
                                  P.op("pe", lambda e, c=c, ui=ui, sl=sl, pb=pb: e.matmul(pu[ui][:, sl, :], kdT[pb:pb + 32, c // 4, :], Vh[pb:pb + 32, c // 4, :], start=True, stop=True, tile_position=(pb, 0)),
                                       reads=["kdT", "Vh"], writes=["hpu%d_%d" % (ui, sl)])
                                if os.environ.get("KSKIP") == "chain":
                                    continue
                                if n >= 8 and os.environ.get("KSKIP") != "actcopy":
                                    P.op("act", lambda e, n=n, c=c: e.activation(Sb[:, c, :], S32[:, n % 4, :], AF.Copy), reads=["S32_%d" % (n % 4)], writes=["Sb"])
                                if n == NCK - 1:
                                    break
                                col = c * CH + di
                                P.op("dve", lambda e, n=n, col=col: e.tensor_scalar(S32[:, (n + 1) % 4, :], S32[:, n % 4, :], B1[:, col:col + 1], None, ALU.mult),
                                     reads=["S32_%d" % (n % 4), "B1"], writes=["S32_%d" % ((n + 1) % 4)])
                                P.op("dve", lambda e, n=n, sl=sl, ui=ui: e.tensor_tensor(S32[:, (n + 1) % 4, :], pu[ui][:, sl, :], S32[:, (n + 1) % 4, :], ALU.add),
                                     reads=["S32_%d" % ((n + 1) % 4), "hpu%d_%d" % (ui, sl)], writes=["S32_%d" % ((n + 1) % 4)])
                        cut(50)
                        for g0 in range(0, 16, 4):
                            pi = pvr[0] % 2
                            pvr[0] += 1
                            po = pv[pi]

                            def fo(e, g0=g0, po=po):
                                ins = None
                                for gi in range(4):
                                    g = g0 + gi
                                    ins = e.matmul(po[:, gi, :], Vh[:, g, :], AT[:, g, :], start=True, stop=False)
                                    for cc in range(4):
                                        c = 4 * g + cc
                                        ins = e.matmul(po[:, gi, cc * CH:(cc + 1) * CH], Sb[:, c, :], qp[:, c * CH:(c + 1) * CH], start=False, stop=(cc == 3))
                                return ins
                            P.op("pe", fo, reads=["Vh", "AT", "Sb", "qp"], writes=["hpv%d" % pi])
                            osl = oacc[:, g0 * 128:(g0 + 4) * 128].rearrange("p (g t) -> p g t", t=128)
                            if dd == 0:
                                P.op("dve", lambda e, osl=osl, po=po: e.tensor_copy(osl, po[:]), reads=["hpv%d" % pi], writes=["oacc"])
                            else:
                                P.op("dve", lambda e, osl=osl, po=po: e.tensor_tensor(osl, po[:], osl, ALU.add), reads=["hpv%d" % pi, "oacc"], writes=["oacc"])
                        if h == 0 and dd == 0 and "of0" in dbg_d:
                            P.dma("sp", lambda e: e.dma_start(out=dbg_d["of0"], in_=oacc[:]), reads=["oacc"], writes=["dbg"])
                    cut(51)
                    wgh = load_wh(7 * 1024 + h * 128)
                    cut(59)
                    for c4 in range(4):
                        rng = slice(c4 * 512, (c4 + 1) * 512)

                        def evg(pap, pk, rng=rng):
                            if os.environ.get("KSKIP") != "gdve":
                                P.op("dve", lambda e: e.tensor_copy(B0[:, rng], pap), reads=[pk], writes=["B0"])
                            if os.environ.get("KSKIP") != "gact":
                                P.op("act", lambda e: e.activation(B2[:, rng], B0[:, rng], AF.Exp, scale=-1.0), reads=["B0"], writes=["B2"])
                        proj_fm(wgh, hxT, "hxT", c4 * 512, 512, evg)
                        cut(60 + c4)
                    P.op("dve", lambda e: e.tensor_scalar(B2[:, 0:T], B2[:, 0:T], 1.0, None, ALU.add), reads=["B2"], writes=["B2"])
                    P.op("dve", lambda e: e.reciprocal(B2[:, 0:T], B2[:, 0:T]), reads=["B2"], writes=["B2"])
                    P.op("pool", lambda e: e.tensor_tensor(B0[:, 0:T], B0[:, 0:T], B2[:, 0:T], ALU.mult), reads=["B0", "B2"], writes=["B0"])
                    cut(52)
                    P.op("act", lambda e: e.activation(B3[:, 0:T], oacc[:], AF.Square), reads=["oacc"], writes=["B3"])
                    cut(53)
                    for c4 in range(4):
                        rng = slice(c4 * 512, (c4 + 1) * 512)

                        def evn(pap, pk, rng=rng):
                            P.op("act", lambda e: e.activation(B1[:, rng], pap, AF.Ln, scale=1.0 / DH, bias=EPS), reads=[pk], writes=["B1"])
                        mm_f32(onesf[:], "onesf", B3[:, rng], "B3", evn)
                    P.op("act", lambda e: e.activation(B1[:, 0:T], B1[:, 0:T], AF.Exp, scale=-0.5), reads=["B1"], writes=["B1"])
                    cut(54)
                    P.op("dve", lambda e: e.tensor_tensor(B3[:, 0:T], oacc[:], B1[:, 0:T], ALU.mult), reads=["oacc", "B1", "B3"], writes=["B3"])
                    P.op("pool", lambda e: e.tensor_scalar(B0[:, 0:T], B0[:, 0:T], hgF[:, 0:1], None, ALU.mult), reads=["B0", "hgF"], writes=["B0"])
                    P.op("dve", lambda e: e.tensor_tensor(qp[:], B3[:, 0:T], B0[:, 0:T], ALU.mult), reads=["B3", "B0"], writes=["qp"])
                    P.dma("sp", lambda e, h=h: e.dma_start(out=cat_d[8 + h], in_=qp[:]), reads=["qp"], writes=["cat_d%d" % (8 + h)])
                    if h == 0 and "hg0" in dbg_d:
                        P.dma("sp", lambda e: e.dma_start(out=dbg_d["hg0"], in_=qp[:]), reads=["qp"], writes=["dbg"])
                    if stop_after == "C2a" and h == 0:
                        break
                P.disabled = False
                P.barrier()
                P.emit_block()
            if stop_after in ("C2", "C2a"):
                return nc

        NSLOT = NE * CAP
        with ExitStack() as stR:
            idx = sb(stR, "idx", [128, 16, 4], I32)
            gk = sb(stR, "gk", [128, 16, 4])
            with ExitStack() as st:
                wo = sb(st, "wo", [128, 16, D], BF16)
                hx2b = sb(st, "hx2b", [128, 16, D], BF16)
                g1b = sb(st, "g1b", [128, D])
                A2b = sb(st, "A2b", [128, D])
                B2b = sb(st, "B2b", [128, D])
                xt = sb(st, "xtD", [128, D])
                x1t = sb(st, "x1t", [128, D])
                xn = sb(st, "xnD", [128, D])
                ct = sb(st, "ct", [128, 16, 128], BF16)
                junk = sb(st, "junkD", [128, D], BF16)
                h2T = sb(st, "h2T", [128, 16, 128])
                wr = sb(st, "wr", [128, 16, NE])
                brb = sb(st, "brb", [128, NE])
                iotaC = sb(st, "iotaC", [128, NE])
                lstr = sb(st, "lstr", [128, 128])
                maskall = sb(st, "maskall", [128, 16, NE])
                lg = sb(st, "lg", [128, NE])
                mx8 = sb(st, "mx8", [128, 8])
                sm = sb(st, "sm", [128, 8])
                ex = sb(st, "ex", [128, NE])
                gfull = sb(st, "gfull", [128, NE])
                slotf = sb(st, "slotf", [128, NE])
                oh = sb(st, "oh", [128, NE])
                t32 = sb(st, "t32", [128, NE])
                slf = sb(st, "slf", [128, 16, 4])
                pjD = [ps(st, "pjD%d" % i, [128, 512]) for i in range(2)]
                tpD = [ps(st, "tpD%d" % i, [128, 4, 128]) for i in range(2)]
                plg = ps(st, "plg", [128, NE])
                ppos = ps(st, "ppos", [128, NE])

                wov = w_out_d.rearrange("(k p) n -> p k n", p=128)
                for dc in range(4):
                    P.dma("pool", lambda e, dc=dc: e.dma_start(out=wo[:, :, dc * 512:(dc + 1) * 512], in_=wov[:, :, dc * 512:(dc + 1) * 512]), writes=["wo"])
                P.dma("sp", lambda e: e.dma_start(out=g1b[:], in_=bc_d[0:1, :].broadcast_to([128, D])), writes=["g1b"])
                P.dma("sp", lambda e: e.dma_start(out=A2b[:], in_=bc_d[1:2, :].broadcast_to([128, D])), writes=["A2b"])
                P.dma("sp", lambda e: e.dma_start(out=B2b[:], in_=bc_d[2:3, :].broadcast_to([128, D])), writes=["B2b"])
                P.dma("sp", lambda e: e.dma_start(out=wr[:], in_=w_r_d.rearrange("(k p) n -> p k n", p=128)), writes=["wr"])
                P.dma("sp", lambda e: e.dma_start(out=brb[:], in_=b_r_d.broadcast_to([128, NE])), writes=["brb"])
                P.dma("sp", lambda e: e.dma_start(out=iotaC[:], in_=iotae_d), writes=["iotaC"])
                P.dma("sp", lambda e: e.dma_start(out=lstr[:], in_=lstrict_d), writes=["lstr"])
                P.op("dve", lambda e: e.tensor_scalar(iotaC[:], iotaC[:], float(CAP), None, ALU.mult), reads=["iotaC"], writes=["iotaC"])
                P.op("pool", lambda e: e.memset(junk[:], 0.0), writes=["junkD"])
                xs_v = xs_d[0:NSLOT + 128, :].rearrange("(j p) d -> j p d", p=128)
                for jj in range(NSLOT // 128 + 1):
                    P.dma("sp", lambda e, jj=jj: e.dma_start(out=xs_v[jj], in_=junk[:]), reads=["junkD"], writes=["xs_d"])

                catv = cat_d.rearrange("k p t -> p k t")
                for i in range(16):
                    tsl = slice(i * 128, (i + 1) * 128)
                    P.dma("sp", lambda e, tsl=tsl: e.dma_start(out=ct[:], in_=catv[:, :, tsl]), writes=["ct"])
                    P.dma("sp", lambda e, tsl=tsl: e.dma_start(out=xt[:], in_=x_d[tsl, :]), writes=["xtD"])
                    if "cat" in dbg_d:
                        P.dma("sp", lambda e, i=i: e.dma_start(out=dbg_d["cat"][i], in_=ct[:]), reads=["ct"], writes=["dbg"])
                    for dc in range(4):
                        pi = dc % 2
                        rng = slice(dc * 512, (dc + 1) * 512)

                        def fo(e, pi=pi, rng=rng):
                            ins = None
                            for k in range(16):
                                ins = e.matmul(pjD[pi][:], ct[:, k, :], wo[:, k, rng], start=(k == 0), stop=(k == 15))
                            return ins
                        P.op("pe", fo, reads=["ct", "wo"], writes=["pjD%d" % pi])
                        P.op("dve", lambda e, pi=pi, rng=rng: e.tensor_tensor(x1t[:, rng], pjD[pi][:], g1b[:, rng], ALU.mult), reads=["pjD%d" % pi, "g1b"], writes=["x1t"])
                    P.op("pool", lambda e: e.tensor_tensor(x1t[:], x1t[:], xt[:], ALU.add), reads=["x1t", "xtD"], writes=["x1t"])
                    P.dma("sp", lambda e, tsl=tsl: e.dma_start(out=x1_d[tsl, :], in_=x1t[:]), reads=["x1t"], writes=["x1_d"])
                    if i == 0 and "x1" in dbg_d:
                        P.dma("sp", lambda e: e.dma_start(out=dbg_d["x1"], in_=x1t[:]), reads=["x1t"], writes=["dbg"])
                    P.op("act", lambda e: e.activation(junk[:], x1t[:], AF.Square, accum_out=sm[:, 0:1]), reads=["x1t"], writes=["junkD", "sm0"])
                    P.op("act", lambda e: e.activation(sm[:, 0:1], sm[:, 0:1], AF.Ln, scale=1.0 / D, bias=EPS), reads=["sm0"], writes=["sm0"])
                    P.op("act", lambda e: e.activation(sm[:, 0:1], sm[:, 0:1], AF.Exp, scale=-0.5), reads=["sm0"], writes=["sm0"])
                    P.op("dve", lambda e: e.tensor_scalar(xn[:], x1t[:], sm[:, 0:1], None, ALU.mult), reads=["x1t", "sm0"], writes=["xnD"])
                    P.op("dve", lambda e: e.tensor_tensor(xn[:], xn[:], A2b[:], ALU.mult), reads=["xnD", "A2b"], writes=["xnD"])
                    P.op("pool", lambda e: e.tensor_tensor(xn[:], xn[:], B2b[:], ALU.add), reads=["xnD", "B2b"], writes=["xnD"])
                    P.op("act", lambda e, i=i: e.activation(hx2b[:, i, :], xn[:], AF.Copy), reads=["xnD"], writes=["hx2b%d" % i])
                    for kg in range(4):
                        tb = kg % 2

                        def trD(e, kg=kg, tb=tb):
                            ins = None
                            for kk in range(4):
                                k = kg * 4 + kk
                                ins = e.transpose(tpD[tb][:, kk, :], xn[:, k * 128:(k + 1) * 128], identf[:])
                            return ins
                        P.op("pe", trD, reads=["xnD", "identf"], writes=["tpD%d" % tb])
                        P.op("dve", lambda e, kg=kg, tb=tb: e.tensor_copy(h2T[:, kg * 4:(kg + 1) * 4, :], tpD[tb][:]), reads=["tpD%d" % tb], writes=["h2T"])

                    def flg(e):
                        ins = None
                        for k in range(16):
                            ins = e.matmul(plg[:], h2T[:, k, :], wr[:, k, :], start=(k == 0), stop=(k == 15))
                        return ins
                    P.op("pe", flg, reads=["h2T", "wr"], writes=["plg"])
                    P.op("dve", lambda e: e.tensor_tensor(lg[:], plg[:], brb[:], ALU.add), reads=["plg", "brb"], writes=["lg"])
                    if i == 0 and "lg" in dbg_d:
                        P.dma("sp", lambda e: e.dma_start(out=dbg_d["lg"], in_=lg[:]), reads=["lg"], writes=["dbg"])
                    P.op("dve", lambda e: e.max(out=mx8[:], in_=lg[:]), reads=["lg"], writes=["mx8"])
                    P.op("dve", lambda e, i=i: e.tensor_scalar(maskall[:, i, :], lg[:], mx8[:, 3:4], None, ALU.is_ge), reads=["lg", "mx8"], writes=["mask%d" % i])
                    P.op("dve", lambda e: e.tensor_scalar(sm[:, 1:2], mx8[:, 0:1], -1.0, None, ALU.mult), reads=["mx8"], writes=["sm1"])
                    P.op("act", lambda e: e.activation(ex[:], lg[:], AF.Exp, bias=sm[:, 1:2]), reads=["lg", "sm1"], writes=["ex"])
                    P.op("dve", lambda e, i=i: e.tensor_tensor(ex[:], ex[:], maskall[:, i, :], ALU.mult), reads=["ex", "mask%d" % i], writes=["ex"])
                    P.op("dve", lambda e: e.tensor_reduce(sm[:, 2:3], ex[:], AX.X, ALU.add), reads=["ex"], writes=["sm2"])
                    P.op("dve", lambda e: e.reciprocal(sm[:, 2:3], sm[:, 2:3]), reads=["sm2"], writes=["sm2"])
                    P.op("dve", lambda e: e.tensor_scalar(gfull[:], ex[:], sm[:, 2:3], None, ALU.mult), reads=["ex", "sm2"], writes=["gfull"])

                    def fpos(e, i=i):
                        ins = e.matmul(ppos[:], lstr[:], maskall[:, i, :], start=True, stop=(i == 0))
                        for j in range(i):
                            ins = e.matmul(ppos[:], onesf[:], maskall[:, j, :], start=False, stop=(j == i - 1))
                        return ins
                    P.op("pe", fpos, reads=["lstr", "onesf"] + ["mask%d" % j for j in range(i + 1)], writes=["ppos"])
                    P.op("dve", lambda e: e.tensor_scalar(slotf[:], ppos[:], float(CAP), None, ALU.min), reads=["ppos"], writes=["slotf"])
                    P.op("dve", lambda e: e.tensor_tensor(slotf[:], slotf[:], iotaC[:], ALU.add), reads=["slotf", "iotaC"], writes=["slotf"])
                    for k in range(4):
                        P.op("dve", lambda e, k=k: e.tensor_scalar(oh[:], lg[:], mx8[:, k:k + 1], None, ALU.is_equal), reads=["lg", "mx8"], writes=["oh"])
                        P.op("dve", lambda e: e.tensor_tensor(t32[:], oh[:], slotf[:], ALU.mult), reads=["oh", "slotf"], writes=["t32"])
                        P.op("dve", lambda e, i=i, k=k: e.tensor_reduce(slf[:, i, k:k + 1], t32[:], AX.X, ALU.add), reads=["t32"], writes=["slf"])
                        P.op("dve", lambda e: e.tensor_tensor(t32[:], oh[:], gfull[:], ALU.mult), reads=["oh", "gfull", "slf"], writes=["t32"])
                        P.op("dve", lambda e, i=i, k=k: e.tensor_reduce(gk[:, i, k:k + 1], t32[:], AX.X, ALU.add), reads=["t32"], writes=["gk"])
                    P.op("dve", lambda e, i=i: e.tensor_copy(idx[:, i, :], slf[:, i, :]), reads=["slf"], writes=["idx"])
                    for k in range(4):
                        P.dma("pool", lambda e, i=i, k=k: e.indirect_dma_start(
                            out=xs_d, out_offset=bass.IndirectOffsetOnAxis(ap=idx[:, i, k:k + 1], axis=0), in_=hx2b[:, i, :], in_offset=None),
                            reads=["idx", "hx2b%d" % i], writes=["xs_d"])
                if "idx" in dbg_d:
                    P.dma("sp", lambda e: e.dma_start(out=dbg_d["idx"], in_=idx[:]), reads=["idx"], writes=["dbg"])
                    if "gk" in dbg_d:
                        P.dma("sp", lambda e: e.dma_start(out=dbg_d["gk"], in_=gk[:]), reads=["gk"], writes=["dbg"])
                P.barrier()
                P.emit_block()
            if stop_after == "E":
                return nc

            NJ = CAP // 128
            with ExitStack() as st:
                bguF = sb(st, "bguF", [128, 32, NE])
                pg = ps(st, "pg", [128, 1024])
                with ExitStack() as st2:
                    bgr = sb(st2, "bgr", [NE, 2 * D])
                    P.dma("sp", lambda e: e.dma_start(out=bgr[:], in_=b_gu_d), writes=["bgr"])
                    pgv = pg[:].rearrange("p (c e) -> p c e", e=NE)

                    def trb(e):
                        ins = None
                        for c in range(32):
                            ins = e.transpose(pgv[:, c, :], bgr[0:NE, c * 128:(c + 1) * 128], identf[0:NE, 0:NE])
                        return ins
                    P.op("pe", trb, reads=["bgr", "identf"], writes=["pg"])
                    P.op("dve", lambda e: e.tensor_copy(bguF[:], pgv), reads=["pg"], writes=["bguF"])
                    P.barrier()
                    P.emit_block()
                xrows2 = [sb(st, "xrows%d" % i, [128, NJ, D], BF16) for i in range(2)]
                XT = sb(st, "XT", [128, 16, CAP], BF16)
                actT = sb(st, "actT", [128, 16, CAP], BF16)
                wg = [sb(st, "wg%d" % i, [128, 16, 256], BF16) for i in range(2)]
                wl = [sb(st, "wl%d" % i, [128, 16, 256], BF16) for i in range(2)]
                wd = [sb(st, "wd%d" % i, [128, 16, 512], BF16) for i in range(2)]
                yout = [sb(st, "yout%d" % i, [128, NJ, 512]) for i in range(2)]
                bdn = sb(st, "bdn", [128, D])
                a_t = sb(st, "a_t", [128, CAP])
                s_t = sb(st, "s_t", [128, CAP], BF16)
                l_t = sb(st, "l_t", [128, CAP])
                ptx = [ps(st, "ptx%d" % i, [128, 4, 128], BF16) for i in range(2)]
                pl = ps(st, "pl", [128, 1024])
                pd = [ps(st, "pd%d" % i, [128, 512]) for i in range(2)]
                txr = [0]

                def load_rows(ex_):
                    r0_ = ex_ * CAP
                    xr = xrows2[ex_ % 2]
                    P.dma("sp", lambda e: e.dma_start(out=xr[:], in_=xs_d[r0_:r0_ + CAP, :].rearrange("(j p) d -> p j d", p=128)), reads=["xs_d"], writes=["xrows%d" % (ex_ % 2)])

                def tr_group(ex_, j, kg):
                    xr = xrows2[ex_ % 2]
                    xkey = "xrows%d" % (ex_ % 2)
                    tb = txr[0] % 2
                    txr[0] += 1

                    def trx(e):
                        ins = None
                        for kk in range(4):
                            k = kg * 4 + kk
                            ins = e.transpose(ptx[tb][:, kk, :], xr[:, j, k * 128:(k + 1) * 128], identb[:])
                        return ins
                    P.op("pe", trx, reads=[xkey, "identb"], writes=["ptx%d" % tb])
                    dst = XT[:, kg * 4:(kg + 1) * 4, j * 128:(j + 1) * 128]
                    if txr[0] % 2 == 0:
                        P.op("act", lambda e: e.activation(dst, ptx[tb][:], AF.Copy), reads=["ptx%d" % tb], writes=["XT"])
                    else:
                        P.op("dve", lambda e: e.tensor_copy(dst, ptx[tb][:]), reads=["ptx%d" % tb], writes=["XT"])

                load_rows(0)
                for j in range(NJ):
                    for kg in range(4):
                        tr_group(0, j, kg)
                for ex_ in range(NE):
                    r0 = ex_ * CAP
                    P.dma("sp", lambda e, ex_=ex_: e.dma_start(out=bdn[:], in_=b_dn_d[ex_:ex_ + 1, :].broadcast_to([128, D])), writes=["bdn"])
                    if ex_ + 1 < NE:
                        load_rows(ex_ + 1)
                    wguv = w_gu_d[ex_].rearrange("(k p) n -> p k n", p=128)
                    for G in range(8):
                        bi = G % 2
                        P.dma("pool", lambda e, G=G, bi=bi, wguv=wguv: e.dma_start(out=wg[bi][:], in_=wguv[:, :, G * 256:(G + 1) * 256]), writes=["wg%d" % bi])
                        P.dma("pool", lambda e, G=G, bi=bi, wguv=wguv: e.dma_start(out=wl[bi][:], in_=wguv[:, :, D + G * 256:D + (G + 1) * 256]), writes=["wl%d" % bi])
                        for cc in range(2):
                            ffc = 2 * G + cc

                            def fgu(e, wt, pt, cc=cc):
                                ins = None
                                for k in range(16):
                                    ins = e.matmul(pt[:, 0:512], wt[:, k, cc * 128:(cc + 1) * 128], XT[:, k, 0:512], start=(k == 0), stop=(k == 15))
                                for k in range(16):
                                    ins = e.matmul(pt[:, 512:CAP], wt[:, k, cc * 128:(cc + 1) * 128], XT[:, k, 512:CAP], start=(k == 0), stop=(k == 15))
                                return ins
                            P.op("pe", lambda e, bi=bi, fgu=fgu: fgu(e, wg[bi], pg), reads=["wg%d" % bi, "XT"], writes=["pg"])
                            P.op("pe", lambda e, bi=bi, fgu=fgu: fgu(e, wl[bi], pl), reads=["wl%d" % bi, "XT"], writes=["pl"])
                            P.op("dve", lambda e, ffc=ffc, ex_=ex_: e.tensor_scalar(a_t[:], pg[:, 0:CAP], bguF[:, ffc, ex_:ex_ + 1], 7.0, ALU.add, ALU.min), reads=["pg", "bguF"], writes=["a_t"])
                            P.op("act", lambda e: e.activation(s_t[:], a_t[:], AF.Sigmoid, scale=1.702), reads=["a_t"], writes=["s_t"])
                            P.op("act", lambda e, ffc=ffc, ex_=ex_: e.activation(l_t[:], pl[:, 0:CAP], AF.Identity, bias=bguF[:, 16 + ffc, ex_:ex_ + 1]), reads=["pl", "bguF"], writes=["l_t"])
                            P.op("dve", lambda e: e.tensor_scalar(l_t[:], l_t[:], -7.0, 7.0, ALU.max, ALU.min), reads=["l_t"], writes=["l_t"])
                            P.op("dve", lambda e: e.tensor_tensor(a_t[:], a_t[:], s_t[:], ALU.mult), reads=["a_t", "s_t"], writes=["a_t"])
                            P.op("dve", lambda e, ffc=ffc: e.scalar_tensor_tensor(actT[:, ffc, :], l_t[:], 1.0, a_t[:], ALU.add, ALU.mult), reads=["l_t", "a_t"], writes=["actT"])
                    wdnv = w_dn_d[ex_].rearrange("(k p) n -> p k n", p=128)
                    trq = [(j, kg) for j in range(NJ) for kg in range(4)] if ex_ + 1 < NE else []
                    for dc in range(4):
                        bi = dc % 2
                        rng = slice(dc * 512, (dc + 1) * 512)
                        P.dma("pool", lambda e, bi=bi, rng=rng, wdnv=wdnv: e.dma_start(out=wd[bi][:], in_=wdnv[:, :, rng]), writes=["wd%d" % bi])
                        for j in range(NJ):
                            pi = j % 2

                            def fdn(e, j=j, pi=pi, bi=bi):
                                ins = None
                                for k in range(16):
                                    ins = e.matmul(pd[pi][:], actT[:, k, j * 128:(j + 1) * 128], wd[bi][:, k, :], start=(k == 0), stop=(k == 15))
                                return ins
                            P.op("pe", fdn, reads=["actT", "wd%d" % bi], writes=["pd%d" % pi])
                            P.op("dve", lambda e, j=j, pi=pi, bi=bi, rng=rng: e.tensor_tensor(yout[bi][:, j, :], pd[pi][:], bdn[:, rng], ALU.add), reads=["pd%d" % pi, "bdn"], writes=["yout%d" % bi])
                            if trq:
                                tj, tkg = trq.pop(0)
                                tr_group(ex_ + 1, tj, tkg)
                        P.dma("sp", lambda e, bi=bi, rng=rng, r0=r0: e.dma_start(out=ys_d[r0:r0 + CAP, rng].rearrange("(j p) d -> p j d", p=128), in_=yout[bi][:]), reads=["yout%d" % bi], writes=["ys_d"])
                        if ex_ == 0 and "y0" in dbg_d:
                            P.dma("sp", lambda e, bi=bi, dc=dc: e.dma_start(out=dbg_d["y0"][dc], in_=yout[bi][:]), reads=["yout%d" % bi], writes=["dbg"])
                    assert not trq
                    if stop_after == "F0":
                        break
                P.barrier()
                P.emit_block()
            if stop_after in ("F", "F0"):
                return nc

            with ExitStack() as st:
                g2b = sb(st, "g2b", [128, D])
                nfb = sb(st, "nfb", [128, D])
                x1g = sb(st, "x1g", [128, D])
                gt = [sb(st, "gt%d" % i, [128, D]) for i in range(8)]
                acc = sb(st, "acc", [128, D])
                tmpg = sb(st, "tmpg", [128, D])
                junk = sb(st, "junkG", [128, D], BF16)
                smg = sb(st, "smg", [128, 2])
                P.dma("sp", lambda e: e.dma_start(out=g2b[:], in_=bc_d[3:4, :].broadcast_to([128, D])), writes=["g2b"])
                P.dma("sp", lambda e: e.dma_start(out=nfb[:], in_=nfin_d.broadcast_to([128, D])), writes=["nfb"])
                P.op("pool", lambda e: e.memset(tmpg[:], 0.0), writes=["tmpg"])
                P.dma("sp", lambda e: e.dma_start(out=ys_d[NSLOT:NSLOT + 128, :], in_=tmpg[:]), reads=["tmpg"], writes=["ys_d"])
                for i in range(16):
                    tsl = slice(i * 128, (i + 1) * 128)
                    P.dma("sp", lambda e, tsl=tsl: e.dma_start(out=x1g[:], in_=x1_d[tsl, :]), reads=["x1_d"], writes=["x1g"])
                    if i == 0:
                        for k in range(4):
                            P.dma("pool", lambda e, k=k: e.indirect_dma_start(
                                out=gt[k][:], out_offset=None, in_=ys_d, in_offset=bass.IndirectOffsetOnAxis(ap=idx[:, 0, k:k + 1], axis=0)),
                                reads=["idx", "ys_d"], writes=["gt%d" % k])
                    if i + 1 < 16:
                        for k in range(4):
                            gn = ((i + 1) * 4 + k) % 8
                            P.dma("pool", lambda e, i=i, k=k, gn=gn: e.indirect_dma_start(
                                out=gt[gn][:], out_offset=None, in_=ys_d, in_offset=bass.IndirectOffsetOnAxis(ap=idx[:, i + 1, k:k + 1], axis=0)),
                                reads=["idx", "ys_d"], writes=["gt%d" % gn])
                    for k in range(4):
                        gi = (i * 4 + k) % 8
                        if k == 0:
                            P.op("dve", lambda e, i=i, k=k, gi=gi: e.tensor_scalar(acc[:], gt[gi][:], gk[:, i, k:k + 1], None, ALU.mult), reads=["gt%d" % gi, "gk"], writes=["acc"])
                        else:
                            P.op("dve", lambda e, i=i, k=k, gi=gi: e.tensor_scalar(tmpg[:], gt[gi][:], gk[:, i, k:k + 1], None, ALU.mult), reads=["gt%d" % gi, "gk"], writes=["tmpg"])
                            P.op("dve", lambda e: e.tensor_tensor(acc[:], acc[:], tmpg[:], ALU.add), reads=["acc", "tmpg"], writes=["acc"])
                    if i == 0 and "moe" in dbg_d:
                        P.dma("sp", lambda e: e.dma_start(out=dbg_d["moe"], in_=acc[:]), reads=["acc"], writes=["dbg"])
                    P.op("dve", lambda e: e.tensor_tensor(acc[:], acc[:], g2b[:], ALU.mult), reads=["acc", "g2b"], writes=["acc"])
                    P.op("dve", lambda e: e.tensor_tensor(acc[:], acc[:], x1g[:], ALU.add), reads=["acc", "x1g"], writes=["acc"])
                    P.op("act", lambda e: e.activation(junk[:], acc[:], AF.Square, accum_out=smg[:, 0:1]), reads=["acc"], writes=["junkG", "smg"])
                    P.op("act", lambda e: e.activation(smg[:, 0:1], smg[:, 0:1], AF.Ln, scale=1.0 / D, bias=EPS), reads=["smg"], writes=["smg"])
                    P.op("act", lambda e: e.activation(smg[:, 0:1], smg[:, 0:1], AF.Exp, scale=-0.5), reads=["smg"], writes=["smg"])
                    P.op("dve", lambda e: e.tensor_scalar(acc[:], acc[:], smg[:, 0:1], None, ALU.mult), reads=["acc", "smg"], writes=["acc"])
                    P.op("dve", lambda e: e.tensor_tensor(acc[:], acc[:], nfb[:], ALU.mult), reads=["acc", "nfb"], writes=["acc"])
                    P.dma("sp", lambda e, tsl=tsl: e.dma_start(out=out_d[tsl, :], in_=acc[:]), reads=["acc"], writes=["out_d"])
                P.barrier()
                P.emit_block()

    return nc


def _prep_inputs(inputs):
    x = np.asarray(inputs["x"], np.float32)
    consts = _host_consts()
    drow, dcol = _na_gather_idx()
    rpb = np.asarray(inputs["rpb"], np.float32)[0]
    nag = np.ascontiguousarray(rpb[:, drow, dcol].reshape(NH, 128, 7, 2, 64).transpose(0, 1, 3, 2, 4))
    shared = dict(
        w_ada=np.ascontiguousarray(inputs["w_ada"][0]),
        b_ada=np.ascontiguousarray(inputs["b_ada"][0][None, :]),
        norm_ffn=np.ascontiguousarray(inputs["norm_ffn"][0][None, :]),
        norm_final=np.ascontiguousarray(np.asarray(inputs["norm_final"])[None, :]),
        w_in=np.ascontiguousarray(inputs["w_in"][0]),
        hg_norm=np.ascontiguousarray(inputs["hg_norm"][0][:, None]),
        nag=nag,
        w_out=np.ascontiguousarray(inputs["w_out"][0]),
        w_router=np.ascontiguousarray(inputs["w_router"][0]),
        b_router=np.ascontiguousarray(inputs["b_router"][0][None, :]),
        w_gu=np.ascontiguousarray(inputs["w_gu"][0]),
        b_gu=np.ascontiguousarray(inputs["b_gu"][0]),
        w_down=np.ascontiguousarray(inputs["w_down"][0]),
        b_down=np.ascontiguousarray(inputs["b_down"][0]),
    )
    shared.update(consts)
    maps = []
    for b in range(x.shape[0]):
        rows8 = np.zeros((8, D), np.float32)
        rows8[0] = inputs["norm_mix"][0]
        rows8[1] = inputs["lb_table"][0]
        rows8[2] = inputs["lb_table"][1]
        rows8[3] = inputs["c"][b]
        rows8[4] = inputs["c_ctx"]
        m = dict(shared)
        m["x"] = np.ascontiguousarray(x[b])
        m["ctx"] = np.ascontiguousarray(inputs["ctx"][b])
        m["rows8"] = rows8
        maps.append(m)
    return maps


def kernel(**inputs):
    maps = _prep_inputs(inputs)
    nc = build()
    res = run_bass_kernel_spmd(nc, maps, core_ids=list(range(len(maps))))
    return np.stack([r["out"] for r in res.results], axis=0)
```
